# Optimizing a Trainium2 kernel written in Bass

```python
import jax, jax.numpy as jnp
from jax import lax
import numpy as np

D_MODEL = 1024
BATCH = 4
SEQ = 4096
DEPTH = 4

CHUNK = 64
D_CONV = D_MODEL
CONV_WIDTH = 31
D_SGU = D_MODEL
SGU_BLOCK = 128
SGU_GROUPS = 8
SGU_GROUP_DIM = D_SGU // SGU_GROUPS
D_IN = 2 * D_CONV + 2 * D_SGU + 2 * D_MODEL
SPLITS = (D_CONV, 2 * D_CONV, 2 * D_CONV + 2 * D_SGU, 2 * D_CONV + 2 * D_SGU + D_MODEL)
N_EXPERTS = 32
TOP_K = 4
D_EXPERT = D_MODEL
SWIGLU_LIMIT = 7.0
SWIGLU_ALPHA = 1.702
MOE_BLOCK = 128
LN_EPS = 1e-5
DEEPNORM_ALPHA = float((2 * DEPTH) ** 0.25)
DEEPNORM_BETA = float((8 * DEPTH) ** -0.25)

kernel_name = "hybrid_conv_sgu_moe_deepnorm_adaln"


def _layer_norm(x, gain=None, bias=None):
    xf = x.astype(jnp.float32)
    mu = jnp.mean(xf, axis=-1, keepdims=True)
    var = jnp.mean(jnp.square(xf - mu), axis=-1, keepdims=True)
    y = (xf - mu) * lax.rsqrt(var + LN_EPS)
    if gain is not None:
        y = y * gain.astype(jnp.float32) + bias.astype(jnp.float32)
    return y.astype(x.dtype)


def _causal_depthwise_conv(x, w, b):
    y = lax.conv_general_dilated(
        x, w[:, None, :], window_strides=(1,), padding=[(CONV_WIDTH - 1, 0)],
        dimension_numbers=('NWC', 'WIO', 'NWC'), feature_group_count=x.shape[-1])
    return y + b


def _spatial_gating(u, v, ln_g, ln_b, w_s, b_s):
    B, S, _ = v.shape
    n_blk = S // SGU_BLOCK
    v = _layer_norm(v, ln_g, ln_b).reshape(B, n_blk, SGU_BLOCK, SGU_GROUPS, SGU_GROUP_DIM)
    chunk_id = jnp.arange(SGU_BLOCK) // CHUNK
    mask = chunk_id[:, None] >= chunk_id[None, :]
    w = jnp.where(mask[None], w_s, 0)
    mixed = jnp.einsum('gij,bnjgc->bnigc', w, v) + jnp.transpose(b_s)[None, None, :, :, None]
    return u * mixed.reshape(B, S, D_SGU)


def _token_mixer(h, w_in, b_in, conv_w, conv_b, ln_a_g, ln_a_b, w_a, b_a,
                 ln_v_g, ln_v_b, w_s, b_s, w_b, b_b, w_out, b_out):
    z = h @ w_in + b_in
    a_val, a_gate, sgu, g_a, g_b = jnp.split(z, SPLITS, axis=-1)
    ya = a_val * jax.nn.sigmoid(a_gate)
    ya = _causal_depthwise_conv(ya, conv_w, conv_b)
    ya = jax.nn.silu(_layer_norm(ya, ln_a_g, ln_a_b))
    ya = ya @ w_a + b_a
    u, v = jnp.split(jax.nn.gelu(sgu, approximate=False), 2, axis=-1)
    yb = _spatial_gating(u, v, ln_v_g, ln_v_b, w_s, b_s) @ w_b + b_b
    m = jax.nn.sigmoid(g_a) * ya + jax.nn.sigmoid(g_b) * yb
    return m @ w_out + b_out


def _moe(h, w_router, b_router, w_gu, b_gu, w_dn, b_dn):
    B, S, D = h.shape
    T = B * S
    xt = h.reshape(T, D)
    logits = (xt @ w_router + b_router).astype(jnp.float32)
    top_logits, top_idx = lax.top_k(logits, TOP_K)
    top_w = jax.nn.softmax(top_logits, axis=-1)
    flat_e = top_idx.reshape(-1)
    order = jnp.argsort(flat_e)
    sorted_e = flat_e[order]
    counts = jnp.bincount(flat_e, length=N_EXPERTS)
    padded = (counts + MOE_BLOCK - 1) // MOE_BLOCK * MOE_BLOCK
    start = jnp.cumsum(counts) - counts
    pad_end = jnp.cumsum(padded)
    pad_start = pad_end - padded
    dest = pad_start[sorted_e] + jnp.arange(T * TOP_K) - start[sorted_e]
    n_slots = T * TOP_K + N_EXPERTS * MOE_BLOCK
    n_blocks = n_slots // MOE_BLOCK
    slot_tok = jnp.full((n_slots,), T, jnp.int32).at[dest].set((order // TOP_K).astype(jnp.int32))
    slot_w = jnp.zeros((n_slots,), jnp.float32).at[dest].set(top_w.reshape(-1)[order])
    block_e = jnp.minimum(
        jnp.searchsorted(pad_end, jnp.arange(n_blocks) * MOE_BLOCK, side='right'), N_EXPERTS - 1)
    xt_pad = jnp.concatenate([xt, jnp.zeros((1, D), xt.dtype)], axis=0)

    def expert_block(args):
        tok, e = args
        xb = xt_pad[tok]
        gate, up = jnp.split(xb @ w_gu[e] + b_gu[e], 2, axis=-1)
        gate = jnp.minimum(gate, SWIGLU_LIMIT)
        up = jnp.clip(up, -SWIGLU_LIMIT, SWIGLU_LIMIT)
        act = (up + 1) * (gate * jax.nn.sigmoid(SWIGLU_ALPHA * gate))
        return act @ w_dn[e] + b_dn[e]

    yb = lax.map(expert_block, (slot_tok.reshape(n_blocks, MOE_BLOCK), block_e))
    y = jnp.zeros((T + 1, D), jnp.float32).at[slot_tok].add(
        yb.reshape(n_slots, D).astype(jnp.float32) * slot_w[:, None])
    return y[:T].reshape(B, S, D).astype(h.dtype)


def _normal(k, shape, scale):
    return jax.random.normal(k, shape, jnp.float32) * scale


def setup_inputs(seed: int = 0) -> dict:
    key = jax.random.key(seed)
    ks = jax.random.split(key, 32)
    L, D = DEPTH, D_MODEL
    return {
        "x": _normal(ks[0], (BATCH, SEQ, D), 1.0),
        "c": _normal(ks[1], (BATCH, D), 1.0),
        "w_mod": _normal(ks[2], (L, D, 6 * D), 0.5 * D ** -0.5),
        "b_mod": _normal(ks[3], (L, 6 * D), 0.02),
        "w_in": _normal(ks[4], (L, D, D_IN), D ** -0.5),
        "b_in": _normal(ks[5], (L, D_IN), 0.02),
        "conv_w": _normal(ks[6], (L, CONV_WIDTH, D_CONV), CONV_WIDTH ** -0.5),
        "conv_b": _normal(ks[7], (L, D_CONV), 0.02),
        "ln_a_g": 1.0 + _normal(ks[8], (L, D_CONV), 0.05),
        "ln_a_b": _normal(ks[9], (L, D_CONV), 0.02),
        "w_a": _normal(ks[10], (L, D_CONV, D), D_CONV ** -0.5),
        "b_a": _normal(ks[11], (L, D), 0.02),
        "ln_v_g": 1.0 + _normal(ks[12], (L, D_SGU), 0.05),
        "ln_v_b": _normal(ks[13], (L, D_SGU), 0.02),
        "w_s": _normal(ks[14], (L, SGU_GROUPS, SGU_BLOCK, SGU_BLOCK), SGU_BLOCK ** -0.5),
        "b_s": 1.0 + _normal(ks[15], (L, SGU_GROUPS, SGU_BLOCK), 0.1),
        "w_b": _normal(ks[16], (L, D_SGU, D), D_SGU ** -0.5),
        "b_b": _normal(ks[17], (L, D), 0.02),
        "w_out": _normal(ks[18], (L, D, D), DEEPNORM_BETA * D ** -0.5),
        "b_out": _normal(ks[19], (L, D), 0.02),
        "post1_g": 1.0 + _normal(ks[20], (L, D), 0.05),
        "post1_b": _normal(ks[21], (L, D), 0.02),
        "w_router": _normal(ks[22], (L, D, N_EXPERTS), D ** -0.5),
        "b_router": _normal(ks[23], (L, N_EXPERTS), 0.01),
        "w_gu": _normal(ks[24], (L, N_EXPERTS, D, 2 * D_EXPERT), D ** -0.5),
        "b_gu": _normal(ks[25], (L, N_EXPERTS, 2 * D_EXPERT), 0.02),
        "w_dn": _normal(ks[26], (L, N_EXPERTS, D_EXPERT, D), DEEPNORM_BETA * D_EXPERT ** -0.5),
        "b_dn": _normal(ks[27], (L, N_EXPERTS, D), 0.02),
        "post2_g": 1.0 + _normal(ks[28], (L, D), 0.05),
        "post2_b": _normal(ks[29], (L, D), 0.02),
    }


def reference(x, c, w_mod, b_mod, w_in, b_in, conv_w, conv_b, ln_a_g, ln_a_b, w_a, b_a,
              ln_v_g, ln_v_b, w_s, b_s, w_b, b_b, w_out, b_out, post1_g, post1_b,
              w_router, b_router, w_gu, b_gu, w_dn, b_dn, post2_g, post2_b):
    cond = jax.nn.silu(c)
    for l in range(DEPTH):
        mod = cond @ w_mod[l] + b_mod[l]
        sh1, sc1, g1, sh2, sc2, g2 = [m[:, None, :] for m in jnp.split(mod, 6, axis=-1)]
        h = _layer_norm(x) * (1 + sc1) + sh1
        y = _token_mixer(h, w_in[l], b_in[l], conv_w[l], conv_b[l], ln_a_g[l], ln_a_b[l],
                         w_a[l], b_a[l], ln_v_g[l], ln_v_b[l], w_s[l], b_s[l], w_b[l], b_b[l],
                         w_out[l], b_out[l])
        x = _layer_norm(DEEPNORM_ALPHA * x + g1 * y, post1_g[l], post1_b[l])
        h = _layer_norm(x) * (1 + sc2) + sh2
        y = _moe(h, w_router[l], b_router[l], w_gu[l], b_gu[l], w_dn[l], b_dn[l])
        x = _layer_norm(DEEPNORM_ALPHA * x + g2 * y, post2_g[l], post2_b[l])
    return x
```

```python
from contextlib import ExitStack
import numpy as np
import concourse.bass as bass
import concourse.mybir as mybir
from concourse.bass_utils import run_bass_kernel_spmd

F32 = mybir.dt.float32
BF16 = mybir.dt.bfloat16
I32 = mybir.dt.int32
U32 = mybir.dt.uint32
AF = mybir.ActivationFunctionType
ALU = mybir.AluOpType


class _Op:
    __slots__ = ("eng", "fn", "dma", "deps_eng", "deps_dma", "milestone", "mval",
                 "epoch", "dsem", "dval", "idx", "prewait", "region")

    def __init__(self, eng, fn, dma):
        self.eng = eng
        self.fn = fn
        self.dma = dma
        self.deps_eng = {}
        self.deps_dma = []
        self.milestone = False
        self.mval = 0
        self.epoch = 0
        self.dsem = None
        self.dval = 0
        self.idx = 0
        self.prewait = None
        self.region = None


class Sched:
    ENGS = ("pe", "act", "dve", "pool", "sp")
    NDS = 8

    def __init__(self, nc):
        self.nc = nc
        self.ops = {e: [] for e in self.ENGS}
        self.last_w = {}
        self.readers = {}
        self.dma_readers = {}
        self.epoch_id = 0
        self.ndma = {e: 0 for e in self.ENGS}
        self.finals = []
        self.eng_sems = []
        self.dma_sems = {}
        self.cur_region = None
        self.regs = {}

    def setup(self, es, n_epochs=1):
        nc = self.nc
        for ep in range(n_epochs):
            self.eng_sems.append({e: es.enter_context(nc.semaphore(f"s_{e}_{ep}")) for e in ("pe", "act", "dve", "pool", "sp")})
        for q in ("sp", "act", "pool"):
            self.dma_sems[q] = [es.enter_context(nc.semaphore(f"d_{q}_{i}")) for i in range(self.NDS)]
        engobj = {"pe": nc.tensor, "act": nc.scalar, "dve": nc.vector, "pool": nc.gpsimd, "sp": nc.sync}
        for e in self.ENGS:
            self.regs[e] = es.enter_context(engobj[e].register("nused_%s" % e))
        self.es = es

    def new_epoch(self):
        self.epoch_id += 1
        assert self.epoch_id < len(self.eng_sems)

    def _dep(self, op, prod, kind):
        if prod is None or prod is op:
            return
        if prod.dma:
            if prod not in op.deps_dma:
                op.deps_dma.append(prod)
            return
        if prod.eng == op.eng and not op.dma:
            if op.eng == "pe":
                return
            if kind == "WAR":
                return
        cur = op.deps_eng.get(prod.eng)
        if cur is None or prod.idx > cur.idx:
            op.deps_eng[prod.eng] = prod

    def op(self, eng, fn, reads=(), writes=(), dma=False):
        o = _Op(eng, fn, dma)
        o.epoch = self.epoch_id
        o.region = self.cur_region
        lst = self.ops[eng]
        o.idx = len(lst)
        for k in reads:
            self._dep(o, self.last_w.get(k), "RAW")
        for k in writes:
            self._dep(o, self.last_w.get(k), "WAW")
            for r in self.readers.get(k, {}).values():
                self._dep(o, r, "WAR")
            for r in self.dma_readers.get(k, ()):
                self._dep(o, r, "WAR")
        for k in reads:
            if dma:
                self.dma_readers.setdefault(k, []).append(o)
            else:
                self.readers.setdefault(k, {})[eng] = o
        for k in writes:
            self.last_w[k] = o
            self.readers[k] = {}
            self.dma_readers[k] = []
        if dma:
            i = self.ndma[eng]
            self.ndma[eng] = i + 1
            o.dsem = self.dma_sems[eng][i % self.NDS]
            o.dval = 16 * (i // self.NDS + 1)
            if i >= self.NDS:
                o.prewait = (o.dsem, 16 * (i // self.NDS))
        lst.append(o)
        return o

    def dma(self, q, out, in_, reads=(), writes=(), **kw):
        eng = {"sp": "sp", "act": "act", "pool": "pool"}[q]
        return self.op(eng, lambda e: e.dma_start(out=out, in_=in_, **kw), reads, writes, dma=True)

    def final_wait(self, eng, dma_ops):
        self.finals.append((eng, list(dma_ops)))

    def emit(self):
        nc = self.nc
        for e in self.ENGS:
            for o in self.ops[e]:
                for p in o.deps_eng.values():
                    p.milestone = True
        for e in self.ENGS:
            cnt = {}
            for o in self.ops[e]:
                if o.milestone and not o.dma:
                    cnt[o.epoch] = cnt.get(o.epoch, 0) + 1
                    o.mval = cnt[o.epoch]
        engobj = {"pe": "tensor", "act": "scalar", "dve": "vector", "pool": "gpsimd", "sp": "sync"}
        finals = self.finals
        sched = self

        def run(ename, eng):
            known = {}

            def wait(sem, val):
                key = id(sem)
                if known.get(key, 0) >= val:
                    return
                known[key] = val
                eng.wait_ge(sem, val)

            def emit_op(o):
                for p in o.deps_eng.values():
                    wait(sched.eng_sems[p.epoch][p.eng], p.mval)
                for p in o.deps_dma:
                    wait(p.dsem, p.dval)
                if o.prewait is not None:
                    wait(*o.prewait)
                ins = o.fn(eng)
                if o.dma:
                    ins.then_inc(o.dsem, 16)
                elif o.milestone:
                    ins.then_inc(sched.eng_sems[o.epoch][ename], 1)

            ops = sched.ops[ename]
            i = 0
            while i < len(ops):
                o = ops[i]
                if o.region is None:
                    emit_op(o)
                    i += 1
                    continue
                j = i
                while j < len(ops) and ops[j].region == o.region:
                    j += 1
                seg = ops[i:j]
                thr = o.region[1]
                snapshot = dict(known)
                reg = sched.regs[ename]
                with eng.If_lt(reg, thr + 1):
                    dcnt = {}
                    for q in seg:
                        if q.dma:
                            k = id(q.dsem)
                            if k not in dcnt:
                                dcnt[k] = [q.dsem, q.dval - 16, 0]
                            dcnt[k][2] += 1
                    for dsem, before, cnt in dcnt.values():
                        if before > 0:
                            eng.wait_ge(dsem, before)
                        eng.sem_inc(dsem, 16 * cnt)
                    ms = [q for q in seg if q.milestone and not q.dma]
                    if ms:
                        own = sched.eng_sems[ms[0].epoch][ename]
                        if ms[0].mval - 1 > 0:
                            eng.wait_ge(own, ms[0].mval - 1)
                        eng.sem_inc(own, len(ms))
                    if not dcnt and not ms:
                        eng.nop()
                with eng.Else():
                    for q in seg:
                        emit_op(q)
                known.clear()
                known.update(snapshot)
                i = j
            for (fe, dops) in finals:
                if fe == ename:
                    for p in dops:
                        wait(p.dsem, p.dval)

        with nc.Block() as block:
            @block.tensor
            def _(eng):
                run("pe", eng)

            @block.scalar
            def _(eng):
                run("act", eng)

            @block.vector
            def _(eng):
                run("dve", eng)

            @block.gpsimd
            def _(eng):
                run("pool", eng)

            @block.sync
            def _(eng):
                run("sp", eng)


D = 1024
KC = 8
NE = 32
CAP = 384
NTHR = 8
NSC = CAP // 128
PW = 512
NSLOT = 7
CONVW = 31
SEQ = 4096
BATCH = 4
TPC = 2048
ALPHA = float(8 ** 0.25)
EPS = 1e-5
GT = 3
SW_A = 1.702
SW_L = 7.0

PP_BIN = 0
PP_CONVW = 48
PP_CONVB = 296
PP_LNAG = 304
PP_LNAB = 312
PP_BA = 320
PP_BB = 328
PP_BOUT = 336
PP_P1G = 344
PP_P1B = 352
PP_P2G = 360
PP_P2B = 368
PP_BMOD = 376
PP_BGU = 424
NPP = 424 + NE * 16
BC_BV = 0
BC_LVG = 1024
BC_LVB = 2048
BC_BR = 3072
NBC = 3072 + NE


def cst_layout(NT):
    o = {}
    c = 0
    for name, n in (("ident", 128), ("ltri", 128), ("thr5", NTHR), ("uex", NE), ("uin", NE), ("sthr", (4 * NT * 128 + CAP - 1) // CAP + NE), ("pidx", 4)):
        o[name] = (c, c + n)
        c += n
    return o, c


class WStream:
    def __init__(self, S, ring, plan, resolve):
        self.S = S
        self.ring = ring
        self.plan = plan
        self.resolve = resolve
        self.n_loaded = 0
        self.n_released = 0
        self.n_got = 0
        self.collect = plan is None
        self.collected = []
        self.dyn = None
        self.dyn_layer = -1
        self.hold_static = False

    def _fill(self):
        while self.n_loaded < len(self.plan) and self.n_loaded - self.n_released < NSLOT:
            i = self.n_loaded
            s = i % NSLOT
            desc = self.plan[i]
            if desc[0] == "dyn" and desc[1] > self.dyn_layer:
                break
            if desc[0] != "dyn" and self.hold_static:
                break
            if desc[0] == "dyn":
                tab, idx = self.dyn(desc)
                self.S.op("pool", lambda e, s=s, tab=tab, idx=idx: e.indirect_dma_start(out=self.ring[:, s, :, :].rearrange("p a b -> p (a b)"), out_offset=None, in_=tab,
                                                                                        in_offset=bass.IndirectOffsetOnAxis(ap=idx, axis=0)),
                          reads=["es_i"], writes=[("ring", s)], dma=True)
            else:
                src = self.resolve(desc)
                self.S.dma("pool", self.ring[:, s, :, :], src, reads=[], writes=[("ring", s)])
            self.n_loaded += 1

    def enable_dyn(self, l, fn):
        self.dyn = fn
        self.dyn_layer = l
        if not self.collect:
            self._fill()

    def start(self):
        if not self.collect:
            self._fill()

    def get(self, desc):
        i = self.n_got
        self.n_got += 1
        if self.collect:
            self.collected.append(desc)
            return i % NSLOT
        assert self.plan[i] == desc, (i, self.plan[i], desc)
        assert i < self.n_loaded, (i, self.n_loaded, self.n_released)
        return i % NSLOT

    def release(self, n=1):
        self.n_released += n
        if not self.collect:
            self._fill()


class Sched2(Sched):
    def __init__(self, nc):
        super().__init__(nc)
        self.bar_ops = []
        self.bar_pending = set()

    def barrier(self):
        ops = []
        for e in self.ENGS:
            comp = [o for o in self.ops[e] if not o.dma]
            if comp:
                ops.append(comp[-1])
            dm = [o for o in self.ops[e] if o.dma]
            ops.extend(dm[-self.NDS:])
        self.bar_ops = ops
        self.bar_pending = set(self.ENGS)

    def op(self, eng, fn, reads=(), writes=(), dma=False):
        o = super().op(eng, fn, reads, writes, dma)
        if eng in self.bar_pending:
            self.bar_pending.discard(eng)
            for p in self.bar_ops:
                if p is o:
                    continue
                if p.dma:
                    if p not in o.deps_dma:
                        o.deps_dma.append(p)
                elif not (p.eng == "pe" and eng == "pe" and not dma):
                    cur = o.deps_eng.get(p.eng)
                    if cur is None or p.idx > cur.idx:
                        o.deps_eng[p.eng] = p
        return o


DEBUG = False


def build_program(NH, L, plan=None):
    NT = TPC // 128 + NH
    NTOK = NT * 128
    groups = [(t0, min(GT, NT - t0)) for t0 in range(0, NT, GT)]
    GM = GT * 128
    CL, NCST = cst_layout(NT)
    nc = bass.Bass("TRN2", target_bir_lowering=False)

    def dram(name, shape, dtype, kind):
        return nc.dram_tensor(name, shape, dtype, kind=kind).ap()

    xT = dram("xT", [D, NTOK], F32, "ExternalInput")
    cvec = dram("cvec", [128, KC], F32, "ExternalInput")
    flag = dram("flag", [128, 1], F32, "ExternalInput")
    cst = dram("cst", [128, NCST], F32, "ExternalInput")
    w_mod = dram("w_mod", [L, D, 6 * D], F32, "ExternalInput")
    w_in = dram("w_in", [L, D, 6 * D], F32, "ExternalInput")
    w_a = dram("w_a", [L, D, D], F32, "ExternalInput")
    w_b = dram("w_b", [L, D, D], F32, "ExternalInput")
    w_out = dram("w_out", [L, D, D], F32, "ExternalInput")
    w_gu = dram("w_gu", [L, NE * 4 * 128, KC, PW], F32, "ExternalInput")
    w_dn = dram("w_dn", [L, NE * 2 * 128, KC, PW], F32, "ExternalInput")
    ppd = dram("pp", [L, 128, NPP], F32, "ExternalInput")
    bcd = dram("bc", [L, 128, NBC], F32, "ExternalInput")
    bdnd = dram("bdn", [L, NE, D], F32, "ExternalInput")
    bsd = dram("bs", [L, 1, D], F32, "ExternalInput")
    wsTd = dram("wsT", [L, 128, 8, 128], F32, "ExternalInput")
    wrd = dram("wr", [L, D, NE], F32, "ExternalInput")
    yT = dram("yT", [D, NTOK], F32, "ExternalOutput")
    xs0 = dram("xs0", [D, NTOK], F32, "Internal")
    xs1 = dram("xs1", [D, NTOK], F32, "Internal")
    Dscr = dram("Dscr", [L, KC, 128, CONVW * 128], BF16, "Internal")
    Hd = dram("Hd", [NTOK, D], BF16, "ExternalOutput" if DEBUG else "Internal")
    NMIN = (4 * NTOK + CAP - 1) // CAP
    NS = NMIN + NE
    Yd = dram("Yd", [NS * CAP, D], F32, "ExternalOutput" if DEBUG else "Internal")
    Xd = dram("Xd", [NS * CAP, D], BF16, "ExternalOutput" if DEBUG else "Internal")
    if DEBUG:
        dbg = dram("dbg", [128, 4096], F32, "ExternalOutput")
    bgud = dram("bgu", [L, NE * 128, 16], F32, "ExternalInput")

    wmap = {"w_mod": w_mod, "w_in": w_in, "w_a": w_a, "w_b": w_b, "w_out": w_out}

    def resolve(desc):
        name, l, e, c0 = desc
        src = wmap[name][l, :, c0:c0 + PW]
        return src.rearrange("(c p) n -> p c n", p=128)

    chain = [("xT", xT)]
    for i in range(2 * L - 1):
        chain.append(("xs%d" % (i % 2), (xs0, xs1)[i % 2]))
    chain.append(("yT", yT))

    def fm(ap):
        return ap.rearrange("(c p) t -> p c t", p=128)

    S = Sched2(nc)
    es = ExitStack()
    with es:
        uniq = [0]

        def sb(name, shape, dtype, stack=es):
            uniq[0] += 1
            return stack.enter_context(nc.sbuf_tensor("%s_%d" % (name, uniq[0]), shape, dtype))

        def pst(name, shape, dtype, stack=es):
            uniq[0] += 1
            return stack.enter_context(nc.psum_tensor("%s_%d" % (name, uniq[0]), shape, dtype))

        S.setup(es, n_epochs=2 * L + 1)
        ring = sb("ring", [128, NSLOT, KC, PW], BF16)
        cst_sb = sb("cst_sb", [128, NCST], F32)
        identb = sb("identb", [128, 128], BF16)
        ltrib = sb("ltrib", [128, 128], BF16)
        onesm = sb("onesm", [128, 128], BF16)
        onesb = sb("onesb", [128, 128], BF16)
        uexb = sb("uexb", [128, NE], BF16)
        uinb = sb("uinb", [128, NE], BF16)
        cond = sb("cond", [128, KC], BF16)
        cv_sb = sb("cv_sb", [128, KC], F32)
        flag_sb = sb("flag_sb", [128, 1], F32)
        pp = sb("pp_sb", [128, NPP], F32)
        mod = sb("mod_sb", [128, 48], F32)
        scp = sb("scp_sb", [128, 16], F32)
        xg = sb("xg", [128, KC, GM], F32)
        F1 = sb("F1", [128, KC, GM], F32)
        B1 = sb("B1", [128, KC, GM], BF16)
        xbr = [sb("xb%d" % i, [128, GM], BF16) for i in range(4)]
        xsqr = [sb("xsq%d" % i, [128, GM], BF16) for i in range(4)]
        mean_sb = sb("mean_sb", [128, GM], F32)
        msq_sb = sb("msq_sb", [128, GM], F32)
        rstd_sb = sb("rstd_sb", [128, GM], F32)
        tmpf = [sb("tmpf%d" % i, [128, 512], F32) for i in range(4)]
        PA = [pst("PA%d" % i, [128, 512], F32) for i in range(4)]
        PST = pst("PST", [128, 2, 512], F32)

        ident_f = cst_sb[:, CL["ident"][0]:CL["ident"][1]]

        WS = WStream(S, ring, plan, resolve)
        rot = {"xb": 0, "pa": 0, "pp2": 0, "tmp": 0}

        def KS(name, n=KC):
            return [(name, i) for i in range(n)]

        def next_tmp():
            i = rot["tmp"] % 4
            rot["tmp"] += 1
            return tmpf[i], ("tmpf", i)

        def pa_pair():
            a = (rot["pp2"] % 2) * 2
            rot["pp2"] += 1
            return (PA[a], ("PA", a)), (PA[a + 1], ("PA", a + 1))

        def pa_one():
            a = rot["pa"] % 4
            rot["pa"] += 1
            return PA[a], ("PA", a)

        def mm_group(ps_ap, ps_key, pairs, reads):
            n = len(pairs)
            for i, (l_, r_) in enumerate(pairs):
                S.op("pe", lambda e, l_=l_, r_=r_, i=i: e.matmul(ps_ap, lhsT=l_, rhs=r_, start=(i == 0), stop=(i == n - 1)),
                     reads=reads, writes=[ps_key])

        def ppc(col, kc):
            return pp[:, col + kc:col + kc + 1]

        S.dma("sp", cst_sb[:], cst, writes=["cst"])
        S.dma("sp", cv_sb[:], cvec, writes=["cv"])
        S.dma("sp", flag_sb[:], flag, writes=["flag"])
        S.op("dve", lambda e: e.tensor_copy(out=identb[:], in_=ident_f), reads=["cst"], writes=["identb"])
        S.op("dve", lambda e: e.tensor_copy(out=ltrib[:], in_=cst_sb[:, CL["ltri"][0]:CL["ltri"][1]]), reads=["cst"], writes=["ltrib"])
        S.op("dve", lambda e: e.tensor_copy(out=uexb[:], in_=cst_sb[:, CL["uex"][0]:CL["uex"][1]]), reads=["cst"], writes=["uexb"])
        S.op("dve", lambda e: e.tensor_copy(out=uinb[:], in_=cst_sb[:, CL["uin"][0]:CL["uin"][1]]), reads=["cst"], writes=["uinb"])
        S.op("dve", lambda e: e.memset(onesm[:], 1.0 / 1024.0), writes=["onesm"])
        S.op("dve", lambda e: e.memset(onesb[:], 1.0), writes=["onesb"])
        S.op("act", lambda e: e.activation(out=cond[:], in_=cv_sb[:], func=AF.Silu), reads=["cv"], writes=["cond"])
        WS.start()

        def layer_norm(srcb, srcn, G, sc_fn, bi_fn, func, dstb, dstn, mid_hook=None):
            for kc in range(KC):
                r = rot["xb"] % 4
                rot["xb"] += 1
                S.op("dve", lambda e, kc=kc, r=r: e.tensor_copy(out=xbr[r][:, :G], in_=srcb[:, kc, :G]),
                     reads=[(srcn, kc)], writes=[("xb", r)])
                S.op("act", lambda e, kc=kc, r=r: e.activation(out=xsqr[r][:, :G], in_=srcb[:, kc, :G], func=AF.Square),
                     reads=[(srcn, kc)], writes=[("xsq", r)])
                S.op("pe", lambda e, kc=kc, r=r: e.matmul(PST[:, 0, :G], lhsT=onesm[:], rhs=xbr[r][:, :G], start=(kc == 0), stop=(kc == KC - 1)),
                     reads=[("xb", r), "onesm"], writes=[("pst", 0)])
                S.op("pe", lambda e, kc=kc, r=r: e.matmul(PST[:, 1, :G], lhsT=onesm[:], rhs=xsqr[r][:, :G], start=(kc == 0), stop=(kc == KC - 1)),
                     reads=[("xsq", r), "onesm"], writes=[("pst", 1)])
            S.op("act", lambda e: e.activation(out=mean_sb[:, :G], in_=PST[:, 0, :G], func=AF.Copy), reads=[("pst", 0)], writes=["mean"])
            S.op("act", lambda e: e.activation(out=msq_sb[:, :G], in_=PST[:, 0, :G], func=AF.Square), reads=[("pst", 0)], writes=["msq"])
            S.op("dve", lambda e: e.scalar_tensor_tensor(out=rstd_sb[:, :G], in0=PST[:, 1, :G], scalar=EPS, in1=msq_sb[:, :G], op0=ALU.add, op1=ALU.subtract),
                 reads=[("pst", 1), "msq"], writes=["rstd"])
            S.op("act", lambda e: e.activation(out=rstd_sb[:, :G], in_=rstd_sb[:, :G], func=AF.Sqrt), reads=["rstd"], writes=["rstd"])
            if mid_hook is not None:
                mid_hook()
            S.op("dve", lambda e: e.reciprocal(out=rstd_sb[:, :G], in_=rstd_sb[:, :G]), reads=["rstd"], writes=["rstd"])
            S.op("dve", lambda e: e.tensor_tensor(out=F1[:, :, :G], in0=srcb[:, :, :G], in1=mean_sb[:, None, :G].to_broadcast([128, KC, G]), op=ALU.subtract),
                 reads=KS(srcn) + ["mean"], writes=KS("F1"))
            S.op("dve", lambda e: e.tensor_tensor(out=F1[:, :, :G], in0=F1[:, :, :G], in1=rstd_sb[:, None, :G].to_broadcast([128, KC, G]), op=ALU.mult),
                 reads=KS("F1") + ["rstd"], writes=KS("F1"))
            for kc in range(KC):
                S.op("act", lambda e, kc=kc: e.activation(out=dstb[:, kc, :G], in_=F1[:, kc, :G], func=func, scale=sc_fn(kc), bias=bi_fn(kc)),
                     reads=[("F1", kc), "pp", "mod"], writes=[(dstn, kc)])


        def mixer_phase(l, srcn, src, dstn, dst):
            S.barrier()
            S.new_epoch()
            with ExitStack() as ps_:
                bc = sb("bc_sb", [128, NBC], F32, ps_)
                wmT = sb("wmT", [128, 8, 128], BF16, ps_)
                bs_f = sb("bs_f", [1, D], F32, ps_)
                bs_hi = sb("bs_hi", [1, D], BF16, ps_)
                bs_lo = sb("bs_lo", [1, D], BF16, ps_)
                ones1 = sb("ones1", [1, 128], BF16, ps_)
                F2 = sb("F2", [128, KC, GM], F32, ps_)
                YA = sb("YA", [128, KC, 32 + GM], BF16, ps_)
                B3 = sb("B3", [128, KC, GM], BF16, ps_)
                B4 = sb("B4", [128, KC, GM], BF16, ps_)
                V = sb("V", [128, GT, D], BF16, ps_)
                vg = [sb("vg%d" % i, [128, D], F32, ps_) for i in range(2)]
                Dg = [sb("Dg%d" % i, [128, CONVW, 128], BF16, ps_) for i in range(2)]
                st6 = sb("st6", [128, 2, 6], F32, ps_)
                mv = sb("mv", [128, 2], F32, ps_)
                rs1 = sb("rs1", [128, 1], F32, ps_)
                PV = pst("PV", [128, D], F32, ps_)
                PVm = PV[:].rearrange("p (g i) -> p g i", i=128)

                S.dma("sp", bc[:], bcd[l], writes=["bc"])
                S.dma("pool", wmT[:], wsTd[l], writes=["wmT"])
                S.op("dve", lambda e: e.memset(wmT[64:128, :, 0:64], 0.0), reads=["wmT"], writes=["wmT"])
                S.dma("sp", bs_f[:], bsd[l], writes=["bs_f"])
                S.op("dve", lambda e: e.tensor_copy(out=bs_hi[:], in_=bs_f[:]), reads=["bs_f"], writes=["bs_hi"])
                S.op("dve", lambda e: e.tensor_tensor(out=bs_lo[:], in0=bs_f[:], in1=bs_hi[:], op=ALU.subtract), reads=["bs_f", "bs_hi"], writes=["bs_lo"])
                S.op("dve", lambda e: e.memset(ones1[:], 1.0), writes=["ones1"])
                S.op("dve", lambda e: e.memset(YA[:, :, 0:32], 0.0), writes=[("YAh", i) for i in range(KC)])
                def build_diag():
                    for oc in range(KC):
                        db = oc % 2
                        for k in range(CONVW):
                            S.op("dve", lambda e, db=db, k=k, oc=oc: e.tensor_scalar(out=Dg[db][:, k, :], in0=identb[:], scalar1=ppc(PP_CONVW + k * 8, oc),
                                                                                     scalar2=None, op0=ALU.mult),
                                 reads=["identb", "pp"], writes=[("Dg", db)])
                        S.dma("sp", Dscr[l, oc], Dg[db][:].rearrange("p k q -> p (k q)"), reads=[("Dg", db)], writes=[("Dscr", oc)])

                def _grp1(gi, t0, nt):
                    G = nt * 128
                    c0 = t0 * 128
                    S.dma("sp", xg[:, :, :G], fm(src)[:, :, c0:c0 + G], reads=[(srcn, gi)], writes=KS("xg"))
                    layer_norm(xg, "xg", G, lambda kc: scp[:, kc:kc + 1], lambda kc: mod[:, kc:kc + 1], AF.Identity, B1, "B1")
                    if gi == 0:
                        build_diag()
                    for half in range(2):
                        sv = WS.get(("w_in", l, None, 0 * D + half * PW))
                        sg_ = WS.get(("w_in", l, None, 1 * D + half * PW))
                        for o4 in range(4):
                            oc = half * 4 + o4
                            (pv, kv), (pg, kg) = pa_pair()
                            mm_group(pv[:, :G], kv, [(ring[:, sv, kc, o4 * 128:(o4 + 1) * 128], B1[:, kc, :G]) for kc in range(KC)], [("ring", sv)] + KS("B1"))
                            mm_group(pg[:, :G], kg, [(ring[:, sg_, kc, o4 * 128:(o4 + 1) * 128], B1[:, kc, :G]) for kc in range(KC)], [("ring", sg_)] + KS("B1"))
                            tb, tk = next_tmp()
                            S.op("act", lambda e, pg=pg, tb=tb, oc=oc: e.activation(out=tb[:, :G], in_=pg[:, :G], func=AF.Sigmoid, bias=ppc(PP_BIN + 8, oc), scale=1.0),
                                 reads=[kg, "pp"], writes=[tk])
                            S.op("dve", lambda e, pv=pv, tb=tb, oc=oc: e.scalar_tensor_tensor(out=YA[:, oc, 32:32 + G], in0=pv[:, :G], scalar=ppc(PP_BIN + 0, oc),
                                                                                                in1=tb[:, :G], op0=ALU.add, op1=ALU.mult),
                                 reads=[kv, tk, "pp"], writes=[("YA", oc)])
                            if gi == 0 and NH > 0:
                                S.op("dve", lambda e, oc=oc: e.tensor_scalar(out=YA[:, oc, 32:32 + 128 * NH], in0=YA[:, oc, 32:32 + 128 * NH],
                                                                              scalar1=flag_sb[:, 0:1], scalar2=None, op0=ALU.mult),
                                     reads=[("YA", oc), "flag"], writes=[("YA", oc)])
                        WS.release(2)
                    for oc in range(KC):
                        db = oc % 2
                        S.dma("sp", Dg[db][:].rearrange("p k q -> p (k q)"), Dscr[l, oc], reads=[("Dscr", oc)], writes=[("Dg", db)])
                        pc, kpc = pa_one()
                        mm_group(pc[:, :G], kpc, [(Dg[db][:, k, :], YA[:, oc, 2 + k:2 + k + G]) for k in range(CONVW)], [("Dg", db), ("YA", oc), ("YAh", oc)])
                        S.op("act", lambda e, pc=pc, oc=oc: e.activation(out=F1[:, oc, :G], in_=pc[:, :G], func=AF.Identity, bias=ppc(PP_CONVB, oc), scale=1.0),
                             reads=[kpc, "pp"], writes=[("F1", oc)])
                        S.op("dve", lambda e, oc=oc: e.tensor_copy(out=YA[:, oc, 0:32], in_=YA[:, oc, G:G + 32]), reads=[("YA", oc)], writes=[("YAh", oc)])
                    def u_branch():
                        for half in range(2):
                            su = WS.get(("w_in", l, None, 2 * D + half * PW))
                            for o4 in range(4):
                                oc = half * 4 + o4
                                pu, ku = pa_one()
                                mm_group(pu[:, :G], ku, [(ring[:, su, kc, o4 * 128:(o4 + 1) * 128], B1[:, kc, :G]) for kc in range(KC)], [("ring", su)] + KS("B1"))
                                S.op("act", lambda e, pu=pu, oc=oc: e.activation(out=B4[:, oc, :G], in_=pu[:, :G], func=AF.Gelu, bias=ppc(PP_BIN + 16, oc), scale=1.0),
                                     reads=[ku, "pp"], writes=[("B4", oc)])
                            WS.release(1)
                    layer_norm(F1, "F1", G, lambda kc: ppc(PP_LNAG, kc), lambda kc: ppc(PP_LNAB, kc), AF.Silu, B3, "B3", mid_hook=u_branch)
                    for half in range(2):
                        sa = WS.get(("w_a", l, None, half * PW))
                        sg_ = WS.get(("w_in", l, None, 4 * D + half * PW))
                        for o4 in range(4):
                            oc = half * 4 + o4
                            (pv, kv), (pg, kg) = pa_pair()
                            mm_group(pv[:, :G], kv, [(ring[:, sa, kc, o4 * 128:(o4 + 1) * 128], B3[:, kc, :G]) for kc in range(KC)], [("ring", sa)] + KS("B3"))
                            mm_group(pg[:, :G], kg, [(ring[:, sg_, kc, o4 * 128:(o4 + 1) * 128], B1[:, kc, :G]) for kc in range(KC)], [("ring", sg_)] + KS("B1"))
                            tb, tk = next_tmp()
                            S.op("act", lambda e, pg=pg, tb=tb, oc=oc: e.activation(out=tb[:, :G], in_=pg[:, :G], func=AF.Sigmoid, bias=ppc(PP_BIN + 32, oc), scale=1.0),
                                 reads=[kg, "pp"], writes=[tk])
                            S.op("dve", lambda e, pv=pv, tb=tb, oc=oc: e.scalar_tensor_tensor(out=F2[:, oc, :G], in0=pv[:, :G], scalar=ppc(PP_BA, oc),
                                                                                                in1=tb[:, :G], op0=ALU.add, op1=ALU.mult),
                                 reads=[kv, tk, "pp"], writes=[("F2", oc)])
                        WS.release(2)
                    sv0 = WS.get(("w_in", l, None, 3 * D))
                    sv1 = WS.get(("w_in", l, None, 3 * D + PW))

                    def vbank(ti, hv):
                        if ti < 2:
                            return PA[2 * ti + hv][:], ("PA", 2 * ti + hv)
                        return PV[:, hv * 512:(hv + 1) * 512], ("PV", hv)

                    for ti in range(nt):
                        for hv, sv in enumerate((sv0, sv1)):
                            bk, kk = vbank(ti, hv)
                            mm_group(bk, kk, [(B1[:, kc, ti * 128:(ti + 1) * 128], ring[:, sv, kc, :]) for kc in range(KC)], [("ring", sv)] + KS("B1"))
                    for ti in range(nt):
                        vb = vg[ti % 2]
                        vk = ("vg", ti % 2)
                        for hv in range(2):
                            bk, kk = vbank(ti, hv)
                            S.op("dve", lambda e, vb=vb, bk=bk, hv=hv: e.tensor_tensor(out=vb[:, hv * 512:(hv + 1) * 512], in0=bk, in1=bc[:, BC_BV + hv * 512:BC_BV + (hv + 1) * 512], op=ALU.add),
                                 reads=[kk, "bc"], writes=[vk])
                        S.op("act", lambda e, vb=vb: e.activation(out=vb[:], in_=vb[:], func=AF.Gelu), reads=[vk], writes=[vk])
                        for hh in range(2):
                            S.op("dve", lambda e, vb=vb, hh=hh: e.bn_stats(out=st6[:, hh, :], in_=vb[:, hh * 512:(hh + 1) * 512]), reads=[vk], writes=[("st6", hh)])
                        S.op("dve", lambda e: e.bn_aggr(out=mv[:], in_=st6[:].rearrange("p a b -> p (a b)")), reads=[("st6", 0), ("st6", 1)], writes=["mv"])
                        S.op("dve", lambda e: e.tensor_scalar(out=rs1[:], in0=mv[:, 1:2], scalar1=EPS, scalar2=None, op0=ALU.add), reads=["mv"], writes=["rs1"])
                        S.op("act", lambda e: e.activation(out=rs1[:], in_=rs1[:], func=AF.Sqrt), reads=["rs1"], writes=["rs1"])
                        S.op("dve", lambda e: e.reciprocal(out=rs1[:], in_=rs1[:]), reads=["rs1"], writes=["rs1"])
                        S.op("dve", lambda e, vb=vb: e.tensor_scalar(out=vb[:], in0=vb[:], scalar1=mv[:, 0:1], scalar2=rs1[:, 0:1], op0=ALU.subtract, op1=ALU.mult),
                             reads=[vk, "mv", "rs1"], writes=[vk])
                        S.op("dve", lambda e, vb=vb: e.tensor_tensor(out=vb[:], in0=vb[:], in1=bc[:, BC_LVG:BC_LVG + D], op=ALU.mult), reads=[vk, "bc"], writes=[vk])
                        S.op("dve", lambda e, vb=vb, ti=ti: e.tensor_tensor(out=V[:, ti, :], in0=vb[:], in1=bc[:, BC_LVB:BC_LVB + D], op=ALU.add),
                             reads=[vk, "bc"], writes=[("V", ti)])
                        for g in range(8):
                            bk, kk = vbank(ti, g // 4)
                            og = bk[:, (g % 4) * 128:(g % 4 + 1) * 128]
                            S.op("pe", lambda e, g=g, ti=ti, og=og: e.matmul(og, lhsT=V[:, ti, g * 128:(g + 1) * 128], rhs=wmT[:, g, :], start=True, stop=False),
                                 reads=[("V", ti), "wmT"], writes=[kk])
                            S.op("pe", lambda e, g=g, og=og: e.matmul(og, lhsT=ones1[:], rhs=bs_hi[:, g * 128:(g + 1) * 128], start=False, stop=False),
                                 reads=["ones1", "bs_hi"], writes=[kk])
                            S.op("pe", lambda e, g=g, og=og: e.matmul(og, lhsT=ones1[:], rhs=bs_lo[:, g * 128:(g + 1) * 128], start=False, stop=True),
                                 reads=["ones1", "bs_lo"], writes=[kk])
                        for hv in range(2):
                            bk, kk = vbank(ti, hv)
                            S.op("dve", lambda e, ti=ti, bk=bk, hv=hv: e.tensor_tensor(out=B3[:, hv * 4:(hv + 1) * 4, ti * 128:(ti + 1) * 128], in0=bk.rearrange("p (g i) -> p g i", i=128),
                                                                                      in1=B4[:, hv * 4:(hv + 1) * 4, ti * 128:(ti + 1) * 128], op=ALU.mult),
                                 reads=[kk] + KS("B4"), writes=KS("B3"))
                    WS.release(2)
                    for half in range(2):
                        sb_ = WS.get(("w_b", l, None, half * PW))
                        sg_ = WS.get(("w_in", l, None, 5 * D + half * PW))
                        for o4 in range(4):
                            oc = half * 4 + o4
                            (pv, kv), (pg, kg) = pa_pair()
                            mm_group(pv[:, :G], kv, [(ring[:, sb_, kc, o4 * 128:(o4 + 1) * 128], B3[:, kc, :G]) for kc in range(KC)], [("ring", sb_)] + KS("B3"))
                            mm_group(pg[:, :G], kg, [(ring[:, sg_, kc, o4 * 128:(o4 + 1) * 128], B1[:, kc, :G]) for kc in range(KC)], [("ring", sg_)] + KS("B1"))
                            tb, tk = next_tmp()
                            tb2, tk2 = next_tmp()
                            S.op("act", lambda e, pg=pg, tb=tb, oc=oc: e.activation(out=tb[:, :G], in_=pg[:, :G], func=AF.Sigmoid, bias=ppc(PP_BIN + 40, oc), scale=1.0),
                                 reads=[kg, "pp"], writes=[tk])
                            S.op("dve", lambda e, pv=pv, tb=tb, tb2=tb2, oc=oc: e.scalar_tensor_tensor(out=tb2[:, :G], in0=pv[:, :G], scalar=ppc(PP_BB, oc),
                                                                                                         in1=tb[:, :G], op0=ALU.add, op1=ALU.mult),
                                 reads=[kv, tk, "pp"], writes=[tk2])
                            S.op("dve", lambda e, tb2=tb2, oc=oc: e.tensor_tensor(out=B4[:, oc, :G], in0=tb2[:, :G], in1=F2[:, oc, :G], op=ALU.add),
                                 reads=[tk2, ("F2", oc)], writes=[("B4", oc)])
                        WS.release(2)
                    for half in range(2):
                        so = WS.get(("w_out", l, None, half * PW))
                        for o4 in range(4):
                            oc = half * 4 + o4
                            po, ko = pa_one()
                            mm_group(po[:, :G], ko, [(ring[:, so, kc, o4 * 128:(o4 + 1) * 128], B4[:, kc, :G]) for kc in range(KC)], [("ring", so)] + KS("B4"))
                            tb, tk = next_tmp()
                            S.op("dve", lambda e, po=po, tb=tb, oc=oc: e.tensor_scalar(out=tb[:, :G], in0=po[:, :G], scalar1=ppc(PP_BOUT, oc), scalar2=mod[:, 16 + oc:17 + oc],
                                                                                        op0=ALU.add, op1=ALU.mult),
                                 reads=[ko, "pp", "mod"], writes=[tk])
                            S.op("dve", lambda e, tb=tb, oc=oc: e.scalar_tensor_tensor(out=F1[:, oc, :G], in0=xg[:, oc, :G], scalar=ALPHA, in1=tb[:, :G],
                                                                                        op0=ALU.mult, op1=ALU.add),
                                 reads=[tk, ("xg", oc)], writes=[("F1", oc)])
                        WS.release(1)
                    layer_norm(F1, "F1", G, lambda kc: ppc(PP_P1G, kc), lambda kc: ppc(PP_P1B, kc), AF.Identity, xg, "xg")
                    S.dma("sp", fm(dst)[:, :, c0:c0 + G], xg[:, :, :G], reads=KS("xg"), writes=[(dstn, gi)])
                for gi, (t0, nt) in enumerate(groups):
                    _grp1(gi, t0, nt)
                S.barrier()

        def moe_phase(l, srcn, src, dstn, dst, fin):
            S.barrier()
            S.new_epoch()
            with ExitStack() as ps_:
                wrb = sb("wrb", [128, KC, NE], BF16, ps_)
                brc = sb("brc", [128, NE], F32, ps_)
                bdn_sb = sb("bdn_sb", [NE, D], F32, ps_)
                htm = [sb("htm%d" % i, [128, D], BF16, ps_) for i in range(2)]
                lg = sb("lg", [128, NT, NE], F32, ps_)
                mx8 = sb("mx8", [128, NT, 8], F32, ps_)
                nmx = sb("nmx", [128, NT], F32, ps_)
                msk = sb("msk", [128, NT, NE], F32, ps_)
                mskb = sb("mskb", [128, NT, NE], BF16, ps_)
                wts = sb("wts", [128, NT, NE], F32, ps_)
                ssum = sb("ssum", [128, NT], F32, ps_)
                gsl = sb("gsl", [128, NT, NE], F32, ps_)
                gs8 = sb("gs8", [128, NT, 8], F32, ps_)
                gs4 = sb("gs4", [128, NT, 4], I32, ps_)
                w4 = sb("w4", [128, NT, 4], F32, ps_)
                eqm = sb("eqm", [128, NT, NE], F32, ps_)
                cmp5 = sb("cmp5", [NE, NTHR], F32, ps_)
                nb1 = sb("nb1", [NE, 1], F32, ps_)
                pcT = sb("pcT", [NE, 128], BF16, ps_)
                offs1 = sb("offs1", [128, NE], F32, ps_)
                cend = sb("cend", [128, NE], F32, ps_)
                escmp = sb("escmp", [128, NS, NE], F32, ps_)
                es_f = sb("es_f", [128, NS], F32, ps_)
                es_t = sb("es_t", [128, NS], F32, ps_)
                nused_i = sb("nused_i", [1, 1], I32, ps_)
                idx_gu = sb("idx_gu", [128, NS, 4], I32, ps_)
                idx_dn = sb("idx_dn", [128, NS, 2], I32, ps_)
                idx_b = sb("idx_b", [128, NS], I32, ps_)
                bgs = [sb("bgs%d" % i, [128, 16], F32, ps_) for i in range(2)]
                bu1s = [sb("bu1s%d" % i, [128, 8], F32, ps_) for i in range(2)]
                Xs = sb("Xs", [128, NSC, D], BF16, ps_)
                XT = sb("XT", [128, KC, CAP], BF16, ps_)
                ACT_ = sb("ACTb", [128, KC, CAP], BF16, ps_)
                Yb = [sb("Yb%d" % i, [128, D], F32, ps_) for i in range(2)]
                G4 = sb("G4", [128, 4, D], F32, ps_)
                ysums = [sb("ysum%d" % i, [128, D], F32, ps_) for i in range(2)]
                wtsT = sb("wtsT", [NE, 128], F32, ps_)
                PX = pst("PX", [128, D], BF16, ps_)
                PM = pst("PM", [128, 512], F32, ps_)
                PMb = PM[:].bitcast(BF16)

                def dyn_piece(kind, s, c0):
                    h = c0 // PW
                    if kind == "gu":
                        return w_gu.rearrange("l r a b -> (l r) (a b)"), idx_gu[:, s, h:h + 1]
                    if kind == "dn":
                        return w_dn.rearrange("l r a b -> (l r) (a b)"), idx_dn[:, s, h:h + 1]
                    return bgud.rearrange("l r j -> (l r) j"), idx_b[:, s:s + 1]

                S.dma("pool", wrb[:], wrd[l].rearrange("(c p) n -> p c n", p=128), writes=["wrb"])
                S.dma("sp", brc[:], bcd[l, :, BC_BR:BC_BR + NE], writes=["brc"])
                S.dma("sp", bdn_sb[:], bdnd[l], writes=["bdn"])

                def _r1(gi, t0, nt):
                    G = nt * 128
                    c0 = t0 * 128
                    S.dma("sp", xg[:, :, :G], fm(src)[:, :, c0:c0 + G], reads=[(srcn, gi)], writes=KS("xg"))
                    layer_norm(xg, "xg", G, lambda kc: scp[:, 8 + kc:9 + kc], lambda kc: mod[:, 24 + kc:25 + kc], AF.Identity, B1, "B1")
                    for ti in range(nt):
                        T = t0 + ti
                        mm_group(PM[:, 0:NE], "PM", [(B1[:, kc, ti * 128:(ti + 1) * 128], wrb[:, kc, :]) for kc in range(KC)], KS("B1") + ["wrb"])
                        S.op("dve", lambda e, T=T: e.tensor_tensor(out=lg[:, T, :], in0=PM[:, 0:NE], in1=brc[:], op=ALU.add), reads=["PM", "brc"], writes=[("lg", T)])
                        for kc in range(KC):
                            S.op("pe", lambda e, kc=kc, ti=ti: e.transpose(out=PX[:, kc * 128:(kc + 1) * 128], in_=B1[:, kc, ti * 128:(ti + 1) * 128], identity=identb[:]),
                                 reads=[("B1", kc), "identb"], writes=["PX", ("PXh", 0), ("PXh", 1)])
                        hb = htm[T % 2]
                        S.op("act", lambda e, hb=hb: e.activation(out=hb[:], in_=PX[:], func=AF.Copy), reads=["PX", ("PXh", 0), ("PXh", 1)], writes=[("htm", T % 2)])
                        S.dma("sp", Hd[T * 128:(T + 1) * 128, :], hb[:], reads=[("htm", T % 2)], writes=[("H", T)])
                for gi, (t0, nt) in enumerate(groups):
                    _r1(gi, t0, nt)

                for T in range(NT):
                    S.op("dve", lambda e, T=T: e.max(out=mx8[:, T, :], in_=lg[:, T, :]), reads=[("lg", T)], writes=[("mx8", T)])
                    S.op("dve", lambda e, T=T: e.tensor_scalar(out=msk[:, T, :], in0=lg[:, T, :], scalar1=mx8[:, T, 3:4], scalar2=None, op0=ALU.is_ge),
                         reads=[("lg", T), ("mx8", T)], writes=[("msk", T)])
                MXA = [("mx8", T) for T in range(NT)]
                MSA = [("msk", T) for T in range(NT)]
                S.op("dve", lambda e: e.tensor_scalar(out=nmx[:], in0=mx8[:, :, 0], scalar1=-1.0, scalar2=None, op0=ALU.mult), reads=MXA, writes=["nmx"])
                for T in range(NT):
                    S.op("act", lambda e, T=T: e.activation(out=wts[:, T, :], in_=lg[:, T, :], func=AF.Exp, bias=nmx[:, T:T + 1], scale=1.0),
                         reads=[("lg", T), "nmx"], writes=[("wts", T)])
                WTA = [("wts", T) for T in range(NT)]
                S.op("dve", lambda e: e.tensor_tensor(out=wts[:], in0=wts[:], in1=msk[:], op=ALU.mult), reads=WTA + MSA, writes=WTA)
                S.op("dve", lambda e: e.tensor_reduce(out=ssum[:], in_=wts[:], axis=mybir.AxisListType.X, op=ALU.add), reads=WTA, writes=["ssum"])
                S.op("dve", lambda e: e.reciprocal(out=ssum[:], in_=ssum[:]), reads=["ssum"], writes=["ssum"])
                S.op("dve", lambda e: e.tensor_tensor(out=wts[:], in0=wts[:], in1=ssum[:, :, None].to_broadcast([128, NT, NE]), op=ALU.mult),
                     reads=WTA + ["ssum"], writes=WTA)
                S.op("dve", lambda e: e.tensor_copy(out=mskb[:], in_=msk[:]), reads=MSA, writes=["mskb"])
                mm_group(PM[0:NE, 128:256], "PMc", [(mskb[:, T, :], onesb[:]) for T in range(NT)], ["mskb", "onesb"])
                S.op("dve", lambda e: e.tensor_scalar(out=cmp5[:], in0=cst_sb[0:NE, CL["thr5"][0]:CL["thr5"][1]], scalar1=PM[0:NE, 128:129], scalar2=None, op0=ALU.is_lt),
                     reads=["PMc", "cst"], writes=["cmp5"])
                S.op("dve", lambda e: e.tensor_reduce(out=nb1[:], in_=cmp5[:], axis=mybir.AxisListType.X, op=ALU.add), reads=["cmp5"], writes=["nb1"])
                S.op("dve", lambda e: e.tensor_scalar(out=pcT[:], in0=onesb[0:NE, :], scalar1=nb1[:, 0:1], scalar2=float(CAP), op0=ALU.mult, op1=ALU.mult),
                     reads=["onesb", "nb1"], writes=["pcT"])
                S.op("pe", lambda e: e.matmul(PM[:, 256:256 + NE], lhsT=pcT[:], rhs=uexb[0:NE, :], start=True, stop=True), reads=["pcT", "uexb"], writes=["PMo"])
                S.op("pe", lambda e: e.matmul(PM[:, 288:288 + NE], lhsT=pcT[:], rhs=uinb[0:NE, :], start=True, stop=True), reads=["pcT", "uinb"], writes=["PMo"])
                S.op("dve", lambda e: e.tensor_scalar(out=offs1[:], in0=PM[:, 256:256 + NE], scalar1=1.0, scalar2=None, op0=ALU.add), reads=["PMo"], writes=["offs1"])
                S.op("act", lambda e: e.activation(out=cend[:], in_=PM[:, 288:288 + NE], func=AF.Copy), reads=["PMo"], writes=["cend"])
                S.op("dve", lambda e: e.tensor_tensor(out=escmp[:], in0=cend[:, None, :].to_broadcast([128, NS, NE]),
                                                       in1=cst_sb[:, CL["sthr"][0]:CL["sthr"][1], None].to_broadcast([128, NS, NE]), op=ALU.is_le),
                     reads=["cend", "cst"], writes=["escmp"])
                S.op("dve", lambda e: e.tensor_reduce(out=es_f[:], in_=escmp[:], axis=mybir.AxisListType.X, op=ALU.add), reads=["escmp"], writes=["es_f"])
                S.op("dve", lambda e: e.tensor_scalar(out=es_f[:], in0=es_f[:], scalar1=float(NE - 1), scalar2=0.0, op0=ALU.min, op1=ALU.max), reads=["es_f"], writes=["es_f"])
                def mkidx(out_ap, mult, base, h):
                    S.op("dve", lambda e: e.tensor_scalar(out=es_t[:], in0=es_f[:], scalar1=float(mult), scalar2=float(base), op0=ALU.mult, op1=ALU.add),
                         reads=["es_f"], writes=["es_t"])
                    S.op("dve", lambda e: e.tensor_tensor(out=out_ap, in0=es_t[:], in1=cst_sb[:, CL["pidx"][0] + h:CL["pidx"][0] + h + 1].to_broadcast([128, NS]), op=ALU.add),
                         reads=["es_t", "cst"], writes=["es_i"])
                for h in range(4):
                    mkidx(idx_gu[:, :, h], 512, l * NE * 4 * 128, h)
                for h in range(2):
                    mkidx(idx_dn[:, :, h], 256, l * NE * 2 * 128, h)
                mkidx(idx_b[:], 128, l * NE * 128, 0)
                WS.enable_dyn(l, lambda desc: dyn_piece(desc[3], desc[2], desc[4]))
                for T in range(NT):
                    pairs = [(onesb[:], mskb[:, T2, :]) for T2 in range(T)] + [(ltrib[:], mskb[:, T, :])]
                    mm_group(PM[:, 0:NE], "PM", pairs, ["onesb", "ltrib", "mskb"])
                    S.op("dve", lambda e, T=T: e.tensor_tensor(out=gsl[:, T, :], in0=PM[:, 0:NE], in1=offs1[:], op=ALU.add), reads=["PM", "offs1"], writes=[("gsl", T)])
                    S.op("dve", lambda e, T=T: e.tensor_tensor(out=gsl[:, T, :], in0=gsl[:, T, :], in1=msk[:, T, :], op=ALU.mult), reads=[("gsl", T), ("msk", T)], writes=[("gsl", T)])
                    S.op("dve", lambda e, T=T: e.max(out=gs8[:, T, :], in_=gsl[:, T, :]), reads=[("gsl", T)], writes=[("gs8", T)])
                GSA = [("gsl", T) for T in range(NT)]
                G8A = [("gs8", T) for T in range(NT)]
                S.op("dve", lambda e: e.tensor_scalar(out=gs4[:], in0=gs8[:, :, 0:4], scalar1=-1.0, scalar2=None, op0=ALU.add), reads=G8A, writes=["gs4"])
                for k in range(4):
                    S.op("dve", lambda e, k=k: e.tensor_tensor(out=eqm[:], in0=gsl[:], in1=gs8[:, :, k:k + 1].to_broadcast([128, NT, NE]), op=ALU.is_equal),
                         reads=GSA + G8A, writes=["eqm"])
                    S.op("dve", lambda e: e.tensor_tensor(out=eqm[:], in0=eqm[:], in1=wts[:], op=ALU.mult), reads=["eqm"] + WTA, writes=["eqm"])
                    S.op("dve", lambda e, k=k: e.tensor_reduce(out=w4[:, :, k], in_=eqm[:], axis=mybir.AxisListType.X, op=ALU.add), reads=["eqm"], writes=[("w4", k)])
                W4A = [("w4", k) for k in range(4)]
                S.op("dve", lambda e: e.tensor_scalar(out=w4[:], in0=w4[:], scalar1=1.0 / SW_A, scalar2=None, op0=ALU.mult), reads=W4A, writes=W4A)

                for T in range(NT):
                    hb = htm[T % 2]
                    S.dma("sp", hb[:], Hd[T * 128:(T + 1) * 128, :], reads=[("H", T)], writes=[("htm", T % 2)])
                    for k in range(4):
                        S.op("pool", lambda e, hb=hb, T=T, k=k: e.indirect_dma_start(out=Xd, out_offset=bass.IndirectOffsetOnAxis(ap=gs4[:, T, k:k + 1], axis=0),
                                                                                      in_=hb[:], in_offset=None),
                             reads=[("htm", T % 2), "gs4"], writes=[("Xd", T, k)], dma=True)

                XDALL = [("Xd", T, k) for T in range(NT) for k in range(4)]

                def load_x(s):
                    S.dma("sp", Xs[:], Xd[s * CAP:(s + 1) * CAP, :].rearrange("(c p) d -> p c d", p=128), reads=XDALL, writes=[("Xs", sc) for sc in range(NSC)])
                    bg = bgs[s % 2]
                    btab, bidx = dyn_piece("b", s, 0)
                    S.op("pool", lambda e, bg=bg, btab=btab, bidx=bidx: e.indirect_dma_start(out=bg[:], out_offset=None, in_=btab,
                                                                                             in_offset=bass.IndirectOffsetOnAxis(ap=bidx, axis=0)),
                         reads=["es_i"], writes=[("bgs", s % 2)], dma=True)
                    b1 = bu1s[s % 2]
                    S.op("dve", lambda e, bg=bg, b1=b1: e.tensor_scalar(out=b1[:], in0=bg[:, 8:16], scalar1=1.0, scalar2=None, op0=ALU.add),
                         reads=[("bgs", s % 2)], writes=[("bu1s", s % 2)])

                def _step(s):
                    bg = bgs[s % 2]
                    b1 = bu1s[s % 2]
                    for kc in range(KC):
                        pxb = PX if kc % 2 == 0 else PMb
                        pxk = [("PXh", 0), ("PXh", 1), "PX"] if kc % 2 == 0 else ["PM", "PMc", "PMo"]
                        for sc in range(NSC):
                            S.op("pe", lambda e, kc=kc, sc=sc, pxb=pxb: e.transpose(out=pxb[:, sc * 128:(sc + 1) * 128], in_=Xs[:, sc, kc * 128:(kc + 1) * 128], identity=identb[:]),
                                 reads=[("Xs", sc), "identb"], writes=pxk)
                        if kc % 2 == 0:
                            S.op("act", lambda e, kc=kc, pxb=pxb: e.activation(out=XT[:, kc, :], in_=pxb[:, 0:CAP], func=AF.Copy), reads=pxk, writes=[("XT", kc)])
                        else:
                            S.op("dve", lambda e, kc=kc, pxb=pxb: e.tensor_copy(out=XT[:, kc, :], in_=pxb[:, 0:CAP]), reads=pxk, writes=[("XT", kc)])
                    if s + 1 < NS:
                        load_x(s + 1)
                    for half in range(2):
                        sg_ = WS.get(("dyn", l, s, "gu", half * PW))
                        su = WS.get(("dyn", l, s, "gu", D + half * PW))
                        for o4 in range(4):
                            j = half * 4 + o4
                            (pg, kg), (pu, ku) = pa_pair()
                            mm_group(pg[:, :CAP], kg, [(ring[:, sg_, kc, o4 * 128:(o4 + 1) * 128], XT[:, kc, :]) for kc in range(KC)], [("ring", sg_)] + KS("XT"))
                            mm_group(pu[:, :CAP], ku, [(ring[:, su, kc, o4 * 128:(o4 + 1) * 128], XT[:, kc, :]) for kc in range(KC)], [("ring", su)] + KS("XT"))
                            tg, tgk = next_tmp()
                            ts_, tsk = next_tmp()
                            tu, tuk = next_tmp()
                            S.op("dve", lambda e, pg=pg, tg=tg, j=j, bg=bg: e.tensor_scalar(out=tg[:, :CAP], in0=pg[:, :CAP], scalar1=bg[:, j:j + 1], scalar2=SW_L, op0=ALU.add, op1=ALU.min),
                                 reads=[kg, ("bgs", s % 2)], writes=[tgk])
                            S.op("act", lambda e, tg=tg, ts_=ts_: e.activation(out=ts_[:, :CAP], in_=tg[:, :CAP], func=AF.Silu, scale=SW_A), reads=[tgk], writes=[tsk])
                            S.op("dve", lambda e, pu=pu, tu=tu, j=j, b1=b1: e.tensor_scalar(out=tu[:, :CAP], in0=pu[:, :CAP], scalar1=b1[:, j:j + 1], scalar2=1.0 - SW_L, op0=ALU.add, op1=ALU.max),
                                 reads=[ku, ("bu1s", s % 2)], writes=[tuk])
                            S.op("dve", lambda e, tu=tu, ts_=ts_, j=j: e.scalar_tensor_tensor(out=ACT_[:, j, :], in0=tu[:, :CAP], scalar=1.0 + SW_L, in1=ts_[:, :CAP], op0=ALU.min, op1=ALU.mult),
                                 reads=[tuk, tsk], writes=[("ACT", j)])
                        WS.release(2)
                    sd0 = WS.get(("dyn", l, s, "dn", 0))
                    sd1 = WS.get(("dyn", l, s, "dn", PW))
                    for sc in range(NSC):
                        yb = Yb[sc % 2]
                        for hd, sd in enumerate((sd0, sd1)):
                            pd, kd = pa_one()
                            mm_group(pd[:], kd, [(ACT_[:, j, sc * 128:(sc + 1) * 128], ring[:, sd, j, :]) for j in range(KC)], [("ring", sd)] + KS("ACT"))
                            if hd == 0:
                                S.op("act", lambda e, pd=pd, yb=yb: e.activation(out=yb[:, 0:512], in_=pd[:], func=AF.Copy), reads=[kd], writes=[("Yb", sc % 2, 0)])
                            else:
                                S.op("dve", lambda e, pd=pd, yb=yb: e.tensor_copy(out=yb[:, 512:1024], in_=pd[:]), reads=[kd], writes=[("Yb", sc % 2, 1)])
                        S.dma("sp", Yd[s * CAP + sc * 128:s * CAP + (sc + 1) * 128, :], yb[:], reads=[("Yb", sc % 2, 0), ("Yb", sc % 2, 1)], writes=[("Y", s, sc)])
                    WS.release(2)
                S.op("dve", lambda e: e.tensor_scalar(out=nused_i[:], in0=cend[0:1, NE - 1:NE], scalar1=1.0 / CAP, scalar2=0.25, op0=ALU.mult, op1=ALU.add),
                     reads=["cend"], writes=["nused"])
                for en in S.ENGS:
                    S.op(en, lambda e, en=en: e.reg_load(S.regs[en], nused_i[0:1, 0:1]), reads=["nused"], writes=[("nused_reg", en)])
                load_x(0)
                WS.hold_static = True
                for s in range(NS):
                    if s >= NMIN:
                        S.cur_region = (("moe", l, s), s)
                    _step(s)
                S.cur_region = None
                WS.hold_static = False
                WS.release(0)

                YALL = [("Y", s, sc) for s in range(NS) for sc in range(NSC)]

                def _r4(gi, t0, nt):
                    G = nt * 128
                    c0 = t0 * 128
                    S.dma("sp", xg[:, :, :G], fm(src)[:, :, c0:c0 + G], reads=[(srcn, gi)], writes=KS("xg"))
                    S.op("act", lambda e: e.activation(out=xg[:, :, :G], in_=xg[:, :, :G], func=AF.Copy, scale=ALPHA), reads=KS("xg"), writes=KS("xg"))
                    for ti in range(nt):
                        T = t0 + ti
                        for k in range(4):
                            S.op("pool", lambda e, T=T, k=k: e.indirect_dma_start(out=G4[:, k, :], out_offset=None, in_=Yd,
                                                                                   in_offset=bass.IndirectOffsetOnAxis(ap=gs4[:, T, k:k + 1], axis=0)),
                                 reads=YALL + ["gs4"], writes=[("G4", k)], dma=True)
                        ysum = ysums[T % 2]
                        yk = ("ysum", T % 2)
                        pbase = (T % 2) * 2
                        S.op("dve", lambda e, T=T, ysum=ysum: e.tensor_scalar(out=ysum[:], in0=G4[:, 0, :], scalar1=w4[:, T, 0:1], scalar2=None, op0=ALU.mult),
                             reads=[("G4", 0)] + W4A, writes=[yk])
                        for k in range(1, 4):
                            S.op("dve", lambda e, T=T, k=k, ysum=ysum: e.scalar_tensor_tensor(out=ysum[:], in0=G4[:, k, :], scalar=w4[:, T, k:k + 1], in1=ysum[:], op0=ALU.mult, op1=ALU.add),
                                 reads=[("G4", k), yk] + W4A, writes=[yk])
                        S.op("pe", lambda e, T=T: e.transpose(out=PM[0:NE, 128:256], in_=wts[:, T, :], identity=ident_f), reads=[("wts", T), "cst"], writes=["PMc"])
                        S.op("act", lambda e: e.activation(out=wtsT[:], in_=PM[0:NE, 128:256], func=AF.Copy), reads=["PMc"], writes=["wtsT"])
                        for kc in range(KC):
                            pq = PA[pbase + kc // 4]
                            pk = ("PA", pbase + kc // 4)
                            S.op("pe", lambda e, kc=kc, pq=pq, ysum=ysum: e.matmul(pq[:, (kc % 4) * 128:(kc % 4 + 1) * 128], lhsT=ysum[:, kc * 128:(kc + 1) * 128], rhs=ident_f, start=True, stop=False),
                                 reads=[yk, "cst"], writes=[pk])
                            S.op("pe", lambda e, kc=kc, pq=pq: e.matmul(pq[:, (kc % 4) * 128:(kc % 4 + 1) * 128], lhsT=bdn_sb[:, kc * 128:(kc + 1) * 128], rhs=wtsT[:], start=False, stop=True),
                                 reads=["bdn", "wtsT"], writes=[pk])
                        for kc in range(KC):
                            pq = PA[pbase + kc // 4]
                            pk = ("PA", pbase + kc // 4)
                            S.op("dve", lambda e, kc=kc, ti=ti, pq=pq: e.scalar_tensor_tensor(out=F1[:, kc, ti * 128:(ti + 1) * 128], in0=pq[:, (kc % 4) * 128:(kc % 4 + 1) * 128],
                                                                                               scalar=mod[:, 40 + kc:41 + kc], in1=xg[:, kc, ti * 128:(ti + 1) * 128], op0=ALU.mult, op1=ALU.add),
                                 reads=[pk, "mod", ("xg", kc)], writes=[("F1", kc)])
                    layer_norm(F1, "F1", G, lambda kc: ppc(PP_P2G, kc), lambda kc: ppc(PP_P2B, kc), AF.Identity, xg, "xg")
                    d_ = S.dma("sp", fm(dst)[:, :, c0:c0 + G], xg[:, :, :G], reads=KS("xg"), writes=[(dstn, gi)])
                    if fin is not None:
                        fin.append(d_)
                for gi, (t0, nt) in enumerate(groups):
                    _r4(gi, t0, nt)
                if DEBUG:
                    dd = []
                    dd.append(S.dma("sp", dbg[:, 0:NS], es_f[:], reads=["es_f"], writes=["dbg0"]))
                    dd.append(S.dma("sp", dbg[:, 64:96], offs1[:], reads=["offs1"], writes=["dbg1"]))
                    dd.append(S.dma("sp", dbg[:, 96:128], cend[:], reads=["cend"], writes=["dbg2"]))
                    dd.append(S.dma("sp", dbg[:, 128:128 + NT * 8], gs8[:].rearrange("p a b -> p (a b)"), reads=G8A, writes=["dbg3"]))
                    dd.append(S.dma("sp", dbg[:, 512:512 + NT * 4], w4[:].rearrange("p a b -> p (a b)"), reads=W4A, writes=["dbg4"]))
                    dd.append(S.dma("sp", dbg[:, 1024:1024 + NT * NE], wts[:].rearrange("p a b -> p (a b)"), reads=WTA, writes=["dbg5"]))
                    dd.append(S.dma("sp", dbg[:, 2048:2048 + NT * NE], gsl[:].rearrange("p a b -> p (a b)"), reads=GSA, writes=["dbg6"]))
                    fin.extend(dd)
                S.barrier()

        fin_dmas = []
        for l in range(L):
            S.barrier()
            S.dma("sp", pp[:], ppd[l], writes=["pp"])
            PMOD = PA[0]
            for hp in range(12):
                s = WS.get(("w_mod", l, None, hp * PW))
                for oc in range(4):
                    col = hp * 4 + oc
                    for kc in range(KC):
                        S.op("pe", lambda e, s=s, oc=oc, kc=kc, col=col: e.matmul(PMOD[:, col:col + 1], lhsT=ring[:, s, kc, oc * 128:(oc + 1) * 128],
                                                                                  rhs=cond[:, kc:kc + 1], start=(kc == 0), stop=(kc == KC - 1)),
                             reads=[("ring", s), "cond"], writes=[("PA", 0)])
                WS.release()
            S.op("dve", lambda e: e.tensor_tensor(out=mod[:], in0=PMOD[:, 0:48], in1=pp[:, PP_BMOD:PP_BMOD + 48], op=ALU.add),
                 reads=[("PA", 0), "pp"], writes=["mod"])
            S.op("dve", lambda e: e.tensor_scalar(out=scp[:, 0:8], in0=mod[:, 8:16], scalar1=1.0, scalar2=None, op0=ALU.add), reads=["mod"], writes=["mod"])
            S.op("dve", lambda e: e.tensor_scalar(out=scp[:, 8:16], in0=mod[:, 32:40], scalar1=1.0, scalar2=None, op0=ALU.add), reads=["mod"], writes=["mod"])

            srcn, src = chain[2 * l]
            dstn, dst = chain[2 * l + 1]
            mixer_phase(l, srcn, src, dstn, dst)
            srcn, src = chain[2 * l + 1]
            dstn, dst = chain[2 * l + 2]
            moe_phase(l, srcn, src, dstn, dst, fin_dmas if l == L - 1 else None)
        S.final_wait("sp", fin_dmas)
        if plan is not None:
            S.emit()
    return nc, WS.collected


def _vec8(v):
    return np.ascontiguousarray(v.reshape(-1, 128).T)


def _consts(NT):
    CL, NCST = cst_layout(NT)
    c = np.zeros((128, NCST), np.float32)
    c[:, CL["ident"][0]:CL["ident"][1]] = np.eye(128, dtype=np.float32)
    c[:, CL["ltri"][0]:CL["ltri"][1]] = np.triu(np.ones((128, 128), np.float32), 1)
    c[:, CL["thr5"][0]:CL["thr5"][1]] = (np.arange(NTHR, dtype=np.float32) * CAP)[None, :]
    c[:NE, CL["uex"][0]:CL["uex"][1]] = np.triu(np.ones((NE, NE), np.float32), 1)
    c[:NE, CL["uin"][0]:CL["uin"][1]] = np.triu(np.ones((NE, NE), np.float32), 0)
    c[:, CL["sthr"][0]:CL["sthr"][1]] = (np.arange((4 * NT * 128 + CAP - 1) // CAP + NE, dtype=np.float32) * CAP)[None, :]
    c[:, CL["pidx"][0]:CL["pidx"][1]] = np.arange(128, dtype=np.float32)[:, None] + 128.0 * np.arange(4, dtype=np.float32)[None, :]
    return c


def _layer_params(inp, ls):
    Ln = len(ls)
    pp = np.zeros((Ln, 128, NPP), np.float32)
    bc = np.zeros((Ln, 128, NBC), np.float32)
    for i, l in enumerate(ls):
        pp[i, :, PP_BIN:PP_BIN + 48] = _vec8(inp["b_in"][l])
        pp[i, :, PP_CONVW:PP_CONVW + 248] = inp["conv_w"][l].reshape(CONVW, 8, 128).transpose(2, 0, 1).reshape(128, 248)
        pp[i, :, PP_CONVB:PP_CONVB + 8] = _vec8(inp["conv_b"][l])
        pp[i, :, PP_LNAG:PP_LNAG + 8] = _vec8(inp["ln_a_g"][l])
        pp[i, :, PP_LNAB:PP_LNAB + 8] = _vec8(inp["ln_a_b"][l])
        pp[i, :, PP_BA:PP_BA + 8] = _vec8(inp["b_a"][l])
        pp[i, :, PP_BB:PP_BB + 8] = _vec8(inp["b_b"][l])
        pp[i, :, PP_BOUT:PP_BOUT + 8] = _vec8(inp["b_out"][l])
        pp[i, :, PP_P1G:PP_P1G + 8] = _vec8(inp["post1_g"][l])
        pp[i, :, PP_P1B:PP_P1B + 8] = _vec8(inp["post1_b"][l])
        pp[i, :, PP_P2G:PP_P2G + 8] = _vec8(inp["post2_g"][l])
        pp[i, :, PP_P2B:PP_P2B + 8] = _vec8(inp["post2_b"][l])
        pp[i, :, PP_BMOD:PP_BMOD + 48] = _vec8(inp["b_mod"][l])
        pp[i, :, PP_BGU:PP_BGU + NE * 16] = inp["b_gu"][l].reshape(NE, 16, 128).transpose(2, 0, 1).reshape(128, NE * 16)
        bc[i, :, BC_BV:BC_BV + D] = inp["b_in"][l][3 * D:4 * D][None, :]
        bc[i, :, BC_LVG:BC_LVG + D] = inp["ln_v_g"][l][None, :]
        bc[i, :, BC_LVB:BC_LVB + D] = inp["ln_v_b"][l][None, :]
        bc[i, :, BC_BR:BC_BR + NE] = inp["b_router"][l][None, :]
    ls = list(ls)
    out = {
        "pp": pp, "bc": bc,
        "bdn": np.ascontiguousarray(inp["b_dn"][ls]),
        "bs": np.ascontiguousarray(inp["b_s"][ls].reshape(Ln, 1, D)),
        "wsT": np.ascontiguousarray(inp["w_s"][ls].transpose(0, 3, 1, 2)),
        "wr": np.ascontiguousarray(inp["w_router"][ls]),
        "bgu": np.ascontiguousarray(inp["b_gu"][ls].reshape(Ln, NE, 16, 128).transpose(0, 1, 3, 2)).reshape(Ln, NE * 128, 16),
        "w_gu": np.ascontiguousarray(inp["w_gu"][ls].reshape(Ln, NE, KC, 128, 4, PW).transpose(0, 1, 4, 3, 2, 5)).reshape(Ln, NE * 4 * 128, KC, PW),
        "w_dn": np.ascontiguousarray(inp["w_dn"][ls].reshape(Ln, NE, KC, 128, 2, PW).transpose(0, 1, 4, 3, 2, 5)).reshape(Ln, NE * 2 * 128, KC, PW),
    }
    for k in ("w_mod", "w_in", "w_a", "w_b", "w_out"):
        out[k] = inp[k][ls[0]:ls[-1] + 1]
    return out


def _shard_x(x, NH, cores):
    NT = TPC // 128 + NH
    outs = []
    for cid in cores:
        b, half = cid // 2, cid % 2
        start = half * TPC
        xt = np.zeros((D, NT * 128), np.float32)
        xt[:, NH * 128:] = x[b, start:start + TPC, :].T
        if half == 1 and NH > 0:
            xt[:, :NH * 128] = x[b, start - NH * 128:start, :].T
        outs.append(xt)
    return outs


_PROG_CACHE = {}


def _get_prog(NH, L):
    key = (NH, L)
    if key not in _PROG_CACHE:
        _, plan = build_program(NH, L, plan=None)
        nc, _ = build_program(NH, L, plan=plan)
        _PROG_CACHE[key] = nc
    return _PROG_CACHE[key]


def run_layers(inp, x, ls, NH, cores):
    NT = TPC // 128 + NH
    nc = _get_prog(NH, len(ls))
    lp = _layer_params(inp, ls)
    cst = _consts(NT)
    xs = _shard_x(x, NH, cores)
    in_maps = []
    for i, cid in enumerate(cores):
        b, half = cid // 2, cid % 2
        m = dict(lp)
        m["xT"] = xs[i]
        m["cvec"] = _vec8(inp["c"][b])
        m["flag"] = np.full((128, 1), float(half), np.float32)
        m["cst"] = cst
        in_maps.append(m)
    res = run_bass_kernel_spmd(nc, in_maps, core_ids=list(range(len(cores))))
    xn = np.zeros_like(x)
    for i, cid in enumerate(cores):
        b, half = cid // 2, cid % 2
        yT = res.results[i]["yT"]
        xn[b, half * TPC:(half + 1) * TPC, :] = yT[:, NH * 128:].T
    return xn


FUSED = True


def kernel(**inputs):
    inp = {k: np.asarray(v) for k, v in inputs.items()}
    x = np.ascontiguousarray(inp["x"], dtype=np.float32)
    cores = list(range(8))
    if FUSED:
        x = run_layers(inp, x, list(range(4)), 2, cores)
    else:
        for l in range(4):
            x = run_layers(inp, x, [l], 1, cores)
    return x.astype(np.float32)
```

```python
from contextlib import ExitStack
import numpy as np
import concourse.bass as bass
import concourse.mybir as mybir
from concourse.bass_utils import run_bass_kernel_spmd

F32 = mybir.dt.float32
BF16 = mybir.dt.bfloat16
I32 = mybir.dt.int32
U32 = mybir.dt.uint32
AF = mybir.ActivationFunctionType
ALU = mybir.AluOpType


class _Op:
    __slots__ = ("eng", "fn", "dma", "deps_eng", "deps_dma", "milestone", "mval",
                 "epoch", "dsem", "dval", "idx", "prewait", "region")

    def __init__(self, eng, fn, dma):
        self.eng = eng
        self.fn = fn
        self.dma = dma
        self.deps_eng = {}
        self.deps_dma = []
        self.milestone = False
        self.mval = 0
        self.epoch = 0
        self.dsem = None
        self.dval = 0
        self.idx = 0
        self.prewait = None
        self.region = None


class Sched:
    ENGS = ("pe", "act", "dve", "pool", "sp")
    NDS = 8

    def __init__(self, nc):
        self.nc = nc
        self.ops = {e: [] for e in self.ENGS}
        self.last_w = {}
        self.readers = {}
        self.dma_readers = {}
        self.epoch_id = 0
        self.ndma = {e: 0 for e in self.ENGS}
        self.finals = []
        self.eng_sems = []
        self.dma_sems = {}
        self.cur_region = None
        self.regs = {}

    def setup(self, es, n_epochs=1):
        nc = self.nc
        for ep in range(n_epochs):
            self.eng_sems.append({e: es.enter_context(nc.semaphore(f"s_{e}_{ep}")) for e in ("pe", "act", "dve", "pool", "sp")})
        for q in ("sp", "act", "pool"):
            self.dma_sems[q] = [es.enter_context(nc.semaphore(f"d_{q}_{i}")) for i in range(self.NDS)]
        engobj = {"pe": nc.tensor, "act": nc.scalar, "dve": nc.vector, "pool": nc.gpsimd, "sp": nc.sync}
        for e in self.ENGS:
            self.regs[e] = es.enter_context(engobj[e].register("nused_%s" % e))
        self.es = es

    def new_epoch(self):
        self.epoch_id += 1
        assert self.epoch_id < len(self.eng_sems)

    def _dep(self, op, prod, kind):
        if prod is None or prod is op:
            return
        if prod.dma:
            if prod not in op.deps_dma:
                op.deps_dma.append(prod)
            return
        if prod.eng == op.eng and not op.dma:
            if op.eng == "pe":
                return
            if kind == "WAR":
                return
        cur = op.deps_eng.get(prod.eng)
        if cur is None or prod.idx > cur.idx:
            op.deps_eng[prod.eng] = prod

    def op(self, eng, fn, reads=(), writes=(), dma=False):
        o = _Op(eng, fn, dma)
        o.epoch = self.epoch_id
        o.region = self.cur_region
        lst = self.ops[eng]
        o.idx = len(lst)
        for k in reads:
            self._dep(o, self.last_w.get(k), "RAW")
        for k in writes:
            self._dep(o, self.last_w.get(k), "WAW")
            for r in self.readers.get(k, {}).values():
                self._dep(o, r, "WAR")
            for r in self.dma_readers.get(k, ()):
                self._dep(o, r, "WAR")
        for k in reads:
            if dma:
                self.dma_readers.setdefault(k, []).append(o)
            else:
                self.readers.setdefault(k, {})[eng] = o
        for k in writes:
            self.last_w[k] = o
            self.readers[k] = {}
            self.dma_readers[k] = []
        if dma:
            i = self.ndma[eng]
            self.ndma[eng] = i + 1
            o.dsem = self.dma_sems[eng][i % self.NDS]
            o.dval = 16 * (i // self.NDS + 1)
            if i >= self.NDS:
                o.prewait = (o.dsem, 16 * (i // self.NDS))
        lst.append(o)
        return o

    def dma(self, q, out, in_, reads=(), writes=(), **kw):
        eng = {"sp": "sp", "act": "act", "pool": "pool"}[q]
        return self.op(eng, lambda e: e.dma_start(out=out, in_=in_, **kw), reads, writes, dma=True)

    def final_wait(self, eng, dma_ops):
        self.finals.append((eng, list(dma_ops)))

    def emit(self):
        nc = self.nc
        for e in self.ENGS:
            for o in self.ops[e]:
                for p in o.deps_eng.values():
                    p.milestone = True
        for e in self.ENGS:
            cnt = {}
            for o in self.ops[e]:
                if o.milestone and not o.dma:
                    cnt[o.epoch] = cnt.get(o.epoch, 0) + 1
                    o.mval = cnt[o.epoch]
        engobj = {"pe": "tensor", "act": "scalar", "dve": "vector", "pool": "gpsimd", "sp": "sync"}
        finals = self.finals
        sched = self

        def run(ename, eng):
            known = {}

            def wait(sem, val):
                key = id(sem)
                if known.get(key, 0) >= val:
                    return
                known[key] = val
                eng.wait_ge(sem, val)

            def emit_op(o):
                for p in o.deps_eng.values():
                    wait(sched.eng_sems[p.epoch][p.eng], p.mval)
                for p in o.deps_dma:
                    wait(p.dsem, p.dval)
                if o.prewait is not None:
                    wait(*o.prewait)
                ins = o.fn(eng)
                if o.dma:
                    ins.then_inc(o.dsem, 16)
                elif o.milestone:
                    ins.then_inc(sched.eng_sems[o.epoch][ename], 1)

            ops = sched.ops[ename]
            i = 0
            while i < len(ops):
                o = ops[i]
                if o.region is None:
                    emit_op(o)
                    i += 1
                    continue
                j = i
                while j < len(ops) and ops[j].region == o.region:
                    j += 1
                seg = ops[i:j]
                thr = o.region[1]
                snapshot = dict(known)
                reg = sched.regs[ename]
                with eng.If_lt(reg, thr + 1):
                    dcnt = {}
                    for q in seg:
                        if q.dma:
                            k = id(q.dsem)
                            if k not in dcnt:
                                dcnt[k] = [q.dsem, q.dval - 16, 0]
                            dcnt[k][2] += 1
                    for dsem, before, cnt in dcnt.values():
                        if before > 0:
                            eng.wait_ge(dsem, before)
                        eng.sem_inc(dsem, 16 * cnt)
                    ms = [q for q in seg if q.milestone and not q.dma]
                    if ms:
                        own = sched.eng_sems[ms[0].epoch][ename]
                        if ms[0].mval - 1 > 0:
                            eng.wait_ge(own, ms[0].mval - 1)
                        eng.sem_inc(own, len(ms))
                    if not dcnt and not ms:
                        eng.nop()
                with eng.Else():
                    for q in seg:
                        emit_op(q)
                known.clear()
                known.update(snapshot)
                i = j
            for (fe, dops) in finals:
                if fe == ename:
                    for p in dops:
                        wait(p.dsem, p.dval)

        with nc.Block() as block:
            @block.tensor
            def _(eng):
                run("pe", eng)

            @block.scalar
            def _(eng):
                run("act", eng)

            @block.vector
            def _(eng):
                run("dve", eng)

            @block.gpsimd
            def _(eng):
                run("pool", eng)

            @block.sync
            def _(eng):
                run("sp", eng)


D = 1024
KC = 8
NE = 32
CAP = 384
NTHR = 8
NSC = CAP // 128
PW = 512
NSLOT = 8
CONVW = 31
SEQ = 4096
BATCH = 4
TPC = 2048
ALPHA = float(8 ** 0.25)
EPS = 1e-5
GT = 3
SW_A = 1.702
SW_L = 7.0

PP_BIN = 0
PP_CONVW = 48
PP_CONVB = 296
PP_LNAG = 304
PP_LNAB = 312
PP_BA = 320
PP_BB = 328
PP_BOUT = 336
PP_P1G = 344
PP_P1B = 352
PP_P2G = 360
PP_P2B = 368
PP_BMOD = 376
PP_BGU = 424
NPP = 424 + NE * 16
BC_BV = 0
BC_LVG = 1024
BC_LVB = 2048
BC_BR = 3072
NBC = 3072 + NE


def cst_layout(NT):
    o = {}
    c = 0
    for name, n in (("ident", 128), ("ltri", 128), ("thr5", NTHR), ("uex", NE), ("uin", NE), ("sthr", (4 * NT * 128 + CAP - 1) // CAP + NE), ("pidx", 4)):
        o[name] = (c, c + n)
        c += n
    return o, c


class WStream:
    def __init__(self, S, ring, plan, resolve):
        self.S = S
        self.ring = ring
        self.plan = plan
        self.resolve = resolve
        self.n_loaded = 0
        self.n_released = 0
        self.n_got = 0
        self.collect = plan is None
        self.collected = []
        self.dyn = None
        self.dyn_layer = -1
        self.hold_static = False

    def _fill(self):
        while self.n_loaded < len(self.plan) and self.n_loaded - self.n_released < NSLOT:
            i = self.n_loaded
            s = i % NSLOT
            desc = self.plan[i]
            if desc[0] == "dyn" and desc[1] > self.dyn_layer:
                break
            if desc[0] != "dyn" and self.hold_static:
                break
            if desc[0] == "dyn":
                tab, idx = self.dyn(desc)
                self.S.op("pool", lambda e, s=s, tab=tab, idx=idx: e.indirect_dma_start(out=self.ring[:, s, :, :].rearrange("p a b -> p (a b)"), out_offset=None, in_=tab,
                                                                                        in_offset=bass.IndirectOffsetOnAxis(ap=idx, axis=0)),
                          reads=["es_i"], writes=[("ring", s)], dma=True)
            else:
                src = self.resolve(desc)
                self.S.dma("pool", self.ring[:, s, :, :], src, reads=[], writes=[("ring", s)])
            self.n_loaded += 1

    def enable_dyn(self, l, fn):
        self.dyn = fn
        self.dyn_layer = l
        if not self.collect:
            self._fill()

    def start(self):
        if not self.collect:
            self._fill()

    def get(self, desc):
        i = self.n_got
        self.n_got += 1
        if self.collect:
            self.collected.append(desc)
            return i % NSLOT
        assert self.plan[i] == desc, (i, self.plan[i], desc)
        assert i < self.n_loaded, (i, self.n_loaded, self.n_released)
        return i % NSLOT

    def release(self, n=1):
        self.n_released += n
        if not self.collect:
            self._fill()


class Sched2(Sched):
    def __init__(self, nc):
        super().__init__(nc)
        self.bar_ops = []
        self.bar_pending = set()

    def barrier(self):
        ops = []
        for e in self.ENGS:
            comp = [o for o in self.ops[e] if not o.dma]
            if comp:
                ops.append(comp[-1])
            dm = [o for o in self.ops[e] if o.dma]
            ops.extend(dm[-self.NDS:])
        self.bar_ops = ops
        self.bar_pending = set(self.ENGS)

    def op(self, eng, fn, reads=(), writes=(), dma=False):
        o = super().op(eng, fn, reads, writes, dma)
        if eng in self.bar_pending:
            self.bar_pending.discard(eng)
            for p in self.bar_ops:
                if p is o:
                    continue
                if p.dma:
                    if p not in o.deps_dma:
                        o.deps_dma.append(p)
                elif not (p.eng == "pe" and eng == "pe" and not dma):
                    cur = o.deps_eng.get(p.eng)
                    if cur is None or p.idx > cur.idx:
                        o.deps_eng[p.eng] = p
        return o


DEBUG = False


def build_program(NH, L, plan=None):
    NT = TPC // 128 + NH
    NTOK = NT * 128
    groups = [(t0, min(GT, NT - t0)) for t0 in range(0, NT, GT)]
    GM = GT * 128
    CL, NCST = cst_layout(NT)
    nc = bass.Bass("TRN2", target_bir_lowering=False)

    def dram(name, shape, dtype, kind):
        return nc.dram_tensor(name, shape, dtype, kind=kind).ap()

    xT = dram("xT", [D, NTOK], F32, "ExternalInput")
    cvec = dram("cvec", [128, KC], F32, "ExternalInput")
    flag = dram("flag", [128, 1], F32, "ExternalInput")
    cst = dram("cst", [128, NCST], F32, "ExternalInput")
    w_mod = dram("w_mod", [L, D, 6 * D], F32, "ExternalInput")
    w_in = dram("w_in", [L, D, 6 * D], F32, "ExternalInput")
    w_a = dram("w_a", [L, D, D], F32, "ExternalInput")
    w_b = dram("w_b", [L, D, D], F32, "ExternalInput")
    w_out = dram("w_out", [L, D, D], F32, "ExternalInput")
    w_gu = dram("w_gu", [L, NE * 4 * 128, KC, PW], F32, "ExternalInput")
    w_dn = dram("w_dn", [L, NE * 2 * 128, KC, PW], F32, "ExternalInput")
    ppd = dram("pp", [L, 128, NPP], F32, "ExternalInput")
    bcd = dram("bc", [L, 128, NBC], F32, "ExternalInput")
    bdnd = dram("bdn", [L, NE, D], F32, "ExternalInput")
    bsd = dram("bs", [L, 1, D], F32, "ExternalInput")
    wsTd = dram("wsT", [L, 128, 8, 128], F32, "ExternalInput")
    wrd = dram("wr", [L, D, NE], F32, "ExternalInput")
    yT = dram("yT", [D, NTOK], F32, "ExternalOutput")
    xs0 = dram("xs0", [D, NTOK], F32, "Internal")
    xs1 = dram("xs1", [D, NTOK], F32, "Internal")
    Dscr = dram("Dscr", [L, KC, 128, CONVW * 128], BF16, "Internal")
    Hd = dram("Hd", [NTOK, D], BF16, "ExternalOutput" if DEBUG else "Internal")
    NMIN = (4 * NTOK + CAP - 1) // CAP
    NS = NMIN + NE
    Yd = dram("Yd", [NS * CAP, D], F32, "ExternalOutput" if DEBUG else "Internal")
    Xd = dram("Xd", [NS * CAP, D], BF16, "ExternalOutput" if DEBUG else "Internal")
    if DEBUG:
        dbg = dram("dbg", [128, 4096], F32, "ExternalOutput")
    bgud = dram("bgu", [L, NE * 128, 16], F32, "ExternalInput")

    wmap = {"w_mod": w_mod, "w_in": w_in, "w_a": w_a, "w_b": w_b, "w_out": w_out}

    def resolve(desc):
        name, l, e, c0 = desc
        src = wmap[name][l, :, c0:c0 + PW]
        return src.rearrange("(c p) n -> p c n", p=128)

    chain = [("xT", xT)]
    for i in range(2 * L - 1):
        chain.append(("xs%d" % (i % 2), (xs0, xs1)[i % 2]))
    chain.append(("yT", yT))

    def fm(ap):
        return ap.rearrange("(c p) t -> p c t", p=128)

    S = Sched2(nc)
    es = ExitStack()
    with es:
        uniq = [0]

        def sb(name, shape, dtype, stack=es):
            uniq[0] += 1
            return stack.enter_context(nc.sbuf_tensor("%s_%d" % (name, uniq[0]), shape, dtype))

        def pst(name, shape, dtype, stack=es):
            uniq[0] += 1
            return stack.enter_context(nc.psum_tensor("%s_%d" % (name, uniq[0]), shape, dtype))

        S.setup(es, n_epochs=2 * L + 1)
        ring = sb("ring", [128, NSLOT, KC, PW], BF16)
        cst_sb = sb("cst_sb", [128, NCST], F32)
        identb = sb("identb", [128, 128], BF16)
        ltrib = sb("ltrib", [128, 128], BF16)
        onesm = sb("onesm", [128, 128], BF16)
        onesb = sb("onesb", [128, 128], BF16)
        uexb = sb("uexb", [128, NE], BF16)
        uinb = sb("uinb", [128, NE], BF16)
        cond = sb("cond", [128, KC], BF16)
        cv_sb = sb("cv_sb", [128, KC], F32)
        flag_sb = sb("flag_sb", [128, 1], F32)
        pp = sb("pp_sb", [128, NPP], F32)
        mod = sb("mod_sb", [128, 48], F32)
        scp = sb("scp_sb", [128, 16], F32)
        xg = sb("xg", [128, KC, GM], F32)
        F1 = sb("F1", [128, KC, GM], F32)
        B1 = sb("B1", [128, KC, GM], BF16)
        xbr = [sb("xb%d" % i, [128, GM], BF16) for i in range(4)]
        xsqr = [sb("xsq%d" % i, [128, GM], BF16) for i in range(4)]
        mean_sb = sb("mean_sb", [128, GM], F32)
        msq_sb = sb("msq_sb", [128, GM], F32)
        rstd_sb = sb("rstd_sb", [128, GM], F32)
        tmpf = [sb("tmpf%d" % i, [128, 512], F32) for i in range(4)]
        PA = [pst("PA%d" % i, [128, 512], F32) for i in range(4)]
        PST = pst("PST", [128, 2, 512], F32)

        ident_f = cst_sb[:, CL["ident"][0]:CL["ident"][1]]

        WS = WStream(S, ring, plan, resolve)
        rot = {"xb": 0, "pa": 0, "pp2": 0, "tmp": 0}

        def KS(name, n=KC):
            return [(name, i) for i in range(n)]

        def next_tmp():
            i = rot["tmp"] % 4
            rot["tmp"] += 1
            return tmpf[i], ("tmpf", i)

        def pa_pair():
            a = (rot["pp2"] % 2) * 2
            rot["pp2"] += 1
            return (PA[a], ("PA", a)), (PA[a + 1], ("PA", a + 1))

        def pa_one():
            a = rot["pa"] % 4
            rot["pa"] += 1
            return PA[a], ("PA", a)

        def mm_group(ps_ap, ps_key, pairs, reads):
            n = len(pairs)
            for i, (l_, r_) in enumerate(pairs):
                S.op("pe", lambda e, l_=l_, r_=r_, i=i: e.matmul(ps_ap, lhsT=l_, rhs=r_, start=(i == 0), stop=(i == n - 1)),
                     reads=reads, writes=[ps_key])

        def ppc(col, kc):
            return pp[:, col + kc:col + kc + 1]

        S.dma("sp", cst_sb[:], cst, writes=["cst"])
        S.dma("sp", cv_sb[:], cvec, writes=["cv"])
        S.dma("sp", flag_sb[:], flag, writes=["flag"])
        S.op("dve", lambda e: e.tensor_copy(out=identb[:], in_=ident_f), reads=["cst"], writes=["identb"])
        S.op("dve", lambda e: e.tensor_copy(out=ltrib[:], in_=cst_sb[:, CL["ltri"][0]:CL["ltri"][1]]), reads=["cst"], writes=["ltrib"])
        S.op("dve", lambda e: e.tensor_copy(out=uexb[:], in_=cst_sb[:, CL["uex"][0]:CL["uex"][1]]), reads=["cst"], writes=["uexb"])
        S.op("dve", lambda e: e.tensor_copy(out=uinb[:], in_=cst_sb[:, CL["uin"][0]:CL["uin"][1]]), reads=["cst"], writes=["uinb"])
        S.op("dve", lambda e: e.memset(onesm[:], 1.0 / 1024.0), writes=["onesm"])
        S.op("dve", lambda e: e.memset(onesb[:], 1.0), writes=["onesb"])
        S.op("act", lambda e: e.activation(out=cond[:], in_=cv_sb[:], func=AF.Silu), reads=["cv"], writes=["cond"])
        WS.start()

        def layer_norm(srcb, srcn, G, sc_fn, bi_fn, func, dstb, dstn, mid_hook=None):
            for kc in range(KC):
                r = rot["xb"] % 4
                rot["xb"] += 1
                S.op("dve", lambda e, kc=kc, r=r: e.tensor_copy(out=xbr[r][:, :G], in_=srcb[:, kc, :G]),
                     reads=[(srcn, kc)], writes=[("xb", r)])
                S.op("act", lambda e, kc=kc, r=r: e.activation(out=xsqr[r][:, :G], in_=srcb[:, kc, :G], func=AF.Square),
                     reads=[(srcn, kc)], writes=[("xsq", r)])
                S.op("pe", lambda e, kc=kc, r=r: e.matmul(PST[:, 0, :G], lhsT=onesm[:], rhs=xbr[r][:, :G], start=(kc == 0), stop=(kc == KC - 1)),
                     reads=[("xb", r), "onesm"], writes=[("pst", 0)])
                S.op("pe", lambda e, kc=kc, r=r: e.matmul(PST[:, 1, :G], lhsT=onesm[:], rhs=xsqr[r][:, :G], start=(kc == 0), stop=(kc == KC - 1)),
                     reads=[("xsq", r), "onesm"], writes=[("pst", 1)])
            S.op("act", lambda e: e.activation(out=mean_sb[:, :G], in_=PST[:, 0, :G], func=AF.Copy), reads=[("pst", 0)], writes=["mean"])
            S.op("act", lambda e: e.activation(out=msq_sb[:, :G], in_=PST[:, 0, :G], func=AF.Square), reads=[("pst", 0)], writes=["msq"])
            S.op("dve", lambda e: e.scalar_tensor_tensor(out=rstd_sb[:, :G], in0=PST[:, 1, :G], scalar=EPS, in1=msq_sb[:, :G], op0=ALU.add, op1=ALU.subtract),
                 reads=[("pst", 1), "msq"], writes=["rstd"])
            S.op("act", lambda e: e.activation(out=rstd_sb[:, :G], in_=rstd_sb[:, :G], func=AF.Sqrt), reads=["rstd"], writes=["rstd"])
            if mid_hook is not None:
                mid_hook()
            S.op("dve", lambda e: e.reciprocal(out=rstd_sb[:, :G], in_=rstd_sb[:, :G]), reads=["rstd"], writes=["rstd"])
            S.op("dve", lambda e: e.tensor_tensor(out=F1[:, :, :G], in0=srcb[:, :, :G], in1=mean_sb[:, None, :G].to_broadcast([128, KC, G]), op=ALU.subtract),
                 reads=KS(srcn) + ["mean"], writes=KS("F1"))
            S.op("dve", lambda e: e.tensor_tensor(out=F1[:, :, :G], in0=F1[:, :, :G], in1=rstd_sb[:, None, :G].to_broadcast([128, KC, G]), op=ALU.mult),
                 reads=KS("F1") + ["rstd"], writes=KS("F1"))
            for kc in range(KC):
                S.op("act", lambda e, kc=kc: e.activation(out=dstb[:, kc, :G], in_=F1[:, kc, :G], func=func, scale=sc_fn(kc), bias=bi_fn(kc)),
                     reads=[("F1", kc), "pp", "mod"], writes=[(dstn, kc)])


        def mixer_phase(l, srcn, src, dstn, dst):
            S.barrier()
            S.new_epoch()
            with ExitStack() as ps_:
                bc = sb("bc_sb", [128, NBC], F32, ps_)
                wmT = sb("wmT", [128, 8, 128], BF16, ps_)
                bs_f = sb("bs_f", [1, D], F32, ps_)
                bs_hi = sb("bs_hi", [1, D], BF16, ps_)
                bs_lo = sb("bs_lo", [1, D], BF16, ps_)
                ones1 = sb("ones1", [1, 128], BF16, ps_)
                F2 = sb("F2", [128, KC, GM], F32, ps_)
                YA = sb("YA", [128, KC, 32 + GM], BF16, ps_)
                B3 = sb("B3", [128, KC, GM], BF16, ps_)
                B4 = sb("B4", [128, KC, GM], BF16, ps_)
                V = sb("V", [128, GT, D], BF16, ps_)
                vg = [sb("vg%d" % i, [128, D], F32, ps_) for i in range(2)]
                Dg = [sb("Dg%d" % i, [128, CONVW, 128], BF16, ps_) for i in range(2)]
                st6 = sb("st6", [128, 2, 6], F32, ps_)
                mv = sb("mv", [128, 2], F32, ps_)
                rs1 = sb("rs1", [128, 1], F32, ps_)
                PV = pst("PV", [128, D], F32, ps_)
                PVm = PV[:].rearrange("p (g i) -> p g i", i=128)

                S.dma("sp", bc[:], bcd[l], writes=["bc"])
                S.dma("pool", wmT[:], wsTd[l], writes=["wmT"])
                S.op("dve", lambda e: e.memset(wmT[64:128, :, 0:64], 0.0), reads=["wmT"], writes=["wmT"])
                S.dma("sp", bs_f[:], bsd[l], writes=["bs_f"])
                S.op("dve", lambda e: e.tensor_copy(out=bs_hi[:], in_=bs_f[:]), reads=["bs_f"], writes=["bs_hi"])
                S.op("dve", lambda e: e.tensor_tensor(out=bs_lo[:], in0=bs_f[:], in1=bs_hi[:], op=ALU.subtract), reads=["bs_f", "bs_hi"], writes=["bs_lo"])
                S.op("dve", lambda e: e.memset(ones1[:], 1.0), writes=["ones1"])
                S.op("dve", lambda e: e.memset(YA[:, :, 0:32], 0.0), writes=[("YAh", i) for i in range(KC)])
                def build_diag():
                    for oc in range(KC):
                        db = oc % 2
                        for k in range(CONVW):
                            S.op("dve", lambda e, db=db, k=k, oc=oc: e.tensor_scalar(out=Dg[db][:, k, :], in0=identb[:], scalar1=ppc(PP_CONVW + k * 8, oc),
                                                                                     scalar2=None, op0=ALU.mult),
                                 reads=["identb", "pp"], writes=[("Dg", db)])
                        S.dma("sp", Dscr[l, oc], Dg[db][:].rearrange("p k q -> p (k q)"), reads=[("Dg", db)], writes=[("Dscr", oc)])

                def _grp1(gi, t0, nt):
                    G = nt * 128
                    c0 = t0 * 128
                    S.dma("sp", xg[:, :, :G], fm(src)[:, :, c0:c0 + G], reads=[(srcn, gi)], writes=KS("xg"))
                    layer_norm(xg, "xg", G, lambda kc: scp[:, kc:kc + 1], lambda kc: mod[:, kc:kc + 1], AF.Identity, B1, "B1")
                    if gi == 0:
                        build_diag()
                    for half in range(2):
                        sv = WS.get(("w_in", l, None, 0 * D + half * PW))
                        sg_ = WS.get(("w_in", l, None, 1 * D + half * PW))
                        for o4 in range(4):
                            oc = half * 4 + o4
                            (pv, kv), (pg, kg) = pa_pair()
                            mm_group(pv[:, :G], kv, [(ring[:, sv, kc, o4 * 128:(o4 + 1) * 128], B1[:, kc, :G]) for kc in range(KC)], [("ring", sv)] + KS("B1"))
                            mm_group(pg[:, :G], kg, [(ring[:, sg_, kc, o4 * 128:(o4 + 1) * 128], B1[:, kc, :G]) for kc in range(KC)], [("ring", sg_)] + KS("B1"))
                            tb, tk = next_tmp()
                            S.op("act", lambda e, pg=pg, tb=tb, oc=oc: e.activation(out=tb[:, :G], in_=pg[:, :G], func=AF.Sigmoid, bias=ppc(PP_BIN + 8, oc), scale=1.0),
                                 reads=[kg, "pp"], writes=[tk])
                            S.op("dve", lambda e, pv=pv, tb=tb, oc=oc: e.scalar_tensor_tensor(out=YA[:, oc, 32:32 + G], in0=pv[:, :G], scalar=ppc(PP_BIN + 0, oc),
                                                                                                in1=tb[:, :G], op0=ALU.add, op1=ALU.mult),
                                 reads=[kv, tk, "pp"], writes=[("YA", oc)])
                            if gi == 0 and NH > 0:
                                S.op("dve", lambda e, oc=oc: e.tensor_scalar(out=YA[:, oc, 32:32 + 128 * NH], in0=YA[:, oc, 32:32 + 128 * NH],
                                                                              scalar1=flag_sb[:, 0:1], scalar2=None, op0=ALU.mult),
                                     reads=[("YA", oc), "flag"], writes=[("YA", oc)])
                        WS.release(2)
                    for oc in range(KC):
                        db = oc % 2
                        S.dma("sp", Dg[db][:].rearrange("p k q -> p (k q)"), Dscr[l, oc], reads=[("Dscr", oc)], writes=[("Dg", db)])
                        pc, kpc = pa_one()
                        mm_group(pc[:, :G], kpc, [(Dg[db][:, k, :], YA[:, oc, 2 + k:2 + k + G]) for k in range(CONVW)], [("Dg", db), ("YA", oc), ("YAh", oc)])
                        S.op("act", lambda e, pc=pc, oc=oc: e.activation(out=F1[:, oc, :G], in_=pc[:, :G], func=AF.Identity, bias=ppc(PP_CONVB, oc), scale=1.0),
                             reads=[kpc, "pp"], writes=[("F1", oc)])
                        S.op("dve", lambda e, oc=oc: e.tensor_copy(out=YA[:, oc, 0:32], in_=YA[:, oc, G:G + 32]), reads=[("YA", oc)], writes=[("YAh", oc)])
                    def u_branch():
                        for half in range(2):
                            su = WS.get(("w_in", l, None, 2 * D + half * PW))
                            for o4 in range(4):
                                oc = half * 4 + o4
                                pu, ku = pa_one()
                                mm_group(pu[:, :G], ku, [(ring[:, su, kc, o4 * 128:(o4 + 1) * 128], B1[:, kc, :G]) for kc in range(KC)], [("ring", su)] + KS("B1"))
                                S.op("act", lambda e, pu=pu, oc=oc: e.activation(out=B4[:, oc, :G], in_=pu[:, :G], func=AF.Gelu, bias=ppc(PP_BIN + 16, oc), scale=1.0),
                                     reads=[ku, "pp"], writes=[("B4", oc)])
                            WS.release(1)
                    layer_norm(F1, "F1", G, lambda kc: ppc(PP_LNAG, kc), lambda kc: ppc(PP_LNAB, kc), AF.Silu, B3, "B3", mid_hook=u_branch)
                    for half in range(2):
                        sa = WS.get(("w_a", l, None, half * PW))
                        sg_ = WS.get(("w_in", l, None, 4 * D + half * PW))
                        for o4 in range(4):
                            oc = half * 4 + o4
                            (pv, kv), (pg, kg) = pa_pair()
                            mm_group(pv[:, :G], kv, [(ring[:, sa, kc, o4 * 128:(o4 + 1) * 128], B3[:, kc, :G]) for kc in range(KC)], [("ring", sa)] + KS("B3"))
                            mm_group(pg[:, :G], kg, [(ring[:, sg_, kc, o4 * 128:(o4 + 1) * 128], B1[:, kc, :G]) for kc in range(KC)], [("ring", sg_)] + KS("B1"))
                            tb, tk = next_tmp()
                            S.op("act", lambda e, pg=pg, tb=tb, oc=oc: e.activation(out=tb[:, :G], in_=pg[:, :G], func=AF.Sigmoid, bias=ppc(PP_BIN + 32, oc), scale=1.0),
                                 reads=[kg, "pp"], writes=[tk])
                            S.op("dve", lambda e, pv=pv, tb=tb, oc=oc: e.scalar_tensor_tensor(out=F2[:, oc, :G], in0=pv[:, :G], scalar=ppc(PP_BA, oc),
                                                                                                in1=tb[:, :G], op0=ALU.add, op1=ALU.mult),
                                 reads=[kv, tk, "pp"], writes=[("F2", oc)])
                        WS.release(2)
                    sv0 = WS.get(("w_in", l, None, 3 * D))
                    sv1 = WS.get(("w_in", l, None, 3 * D + PW))

                    def vbank(ti, hv):
                        if ti < 2:
                            return PA[2 * ti + hv][:], ("PA", 2 * ti + hv)
                        return PV[:, hv * 512:(hv + 1) * 512], ("PV", hv)

                    for ti in range(nt):
                        for hv, sv in enumerate((sv0, sv1)):
                            bk, kk = vbank(ti, hv)
                            mm_group(bk, kk, [(B1[:, kc, ti * 128:(ti + 1) * 128], ring[:, sv, kc, :]) for kc in range(KC)], [("ring", sv)] + KS("B1"))
                    for ti in range(nt):
                        vb = vg[ti % 2]
                        vk = ("vg", ti % 2)
                        for hv in range(2):
                            bk, kk = vbank(ti, hv)
                            S.op("dve", lambda e, vb=vb, bk=bk, hv=hv: e.tensor_tensor(out=vb[:, hv * 512:(hv + 1) * 512], in0=bk, in1=bc[:, BC_BV + hv * 512:BC_BV + (hv + 1) * 512], op=ALU.add),
                                 reads=[kk, "bc"], writes=[vk])
                        S.op("act", lambda e, vb=vb: e.activation(out=vb[:], in_=vb[:], func=AF.Gelu), reads=[vk], writes=[vk])
                        for hh in range(2):
                            S.op("dve", lambda e, vb=vb, hh=hh: e.bn_stats(out=st6[:, hh, :], in_=vb[:, hh * 512:(hh + 1) * 512]), reads=[vk], writes=[("st6", hh)])
                        S.op("dve", lambda e: e.bn_aggr(out=mv[:], in_=st6[:].rearrange("p a b -> p (a b)")), reads=[("st6", 0), ("st6", 1)], writes=["mv"])
                        S.op("dve", lambda e: e.tensor_scalar(out=rs1[:], in0=mv[:, 1:2], scalar1=EPS, scalar2=None, op0=ALU.add), reads=["mv"], writes=["rs1"])
                        S.op("act", lambda e: e.activation(out=rs1[:], in_=rs1[:], func=AF.Sqrt), reads=["rs1"], writes=["rs1"])
                        S.op("dve", lambda e: e.reciprocal(out=rs1[:], in_=rs1[:]), reads=["rs1"], writes=["rs1"])
                        S.op("dve", lambda e, vb=vb: e.tensor_scalar(out=vb[:], in0=vb[:], scalar1=mv[:, 0:1], scalar2=rs1[:, 0:1], op0=ALU.subtract, op1=ALU.mult),
                             reads=[vk, "mv", "rs1"], writes=[vk])
                        S.op("dve", lambda e, vb=vb: e.tensor_tensor(out=vb[:], in0=vb[:], in1=bc[:, BC_LVG:BC_LVG + D], op=ALU.mult), reads=[vk, "bc"], writes=[vk])
                        S.op("dve", lambda e, vb=vb, ti=ti: e.tensor_tensor(out=V[:, ti, :], in0=vb[:], in1=bc[:, BC_LVB:BC_LVB + D], op=ALU.add),
                             reads=[vk, "bc"], writes=[("V", ti)])
                        for g in range(8):
                            bk, kk = vbank(ti, g // 4)
                            og = bk[:, (g % 4) * 128:(g % 4 + 1) * 128]
                            S.op("pe", lambda e, g=g, ti=ti, og=og: e.matmul(og, lhsT=V[:, ti, g * 128:(g + 1) * 128], rhs=wmT[:, g, :], start=True, stop=False),
                                 reads=[("V", ti), "wmT"], writes=[kk])
                            S.op("pe", lambda e, g=g, og=og: e.matmul(og, lhsT=ones1[:], rhs=bs_hi[:, g * 128:(g + 1) * 128], start=False, stop=False),
                                 reads=["ones1", "bs_hi"], writes=[kk])
                            S.op("pe", lambda e, g=g, og=og: e.matmul(og, lhsT=ones1[:], rhs=bs_lo[:, g * 128:(g + 1) * 128], start=False, stop=True),
                                 reads=["ones1", "bs_lo"], writes=[kk])
                        for hv in range(2):
                            bk, kk = vbank(ti, hv)
                            S.op("dve", lambda e, ti=ti, bk=bk, hv=hv: e.tensor_tensor(out=B3[:, hv * 4:(hv + 1) * 4, ti * 128:(ti + 1) * 128], in0=bk.rearrange("p (g i) -> p g i", i=128),
                                                                                      in1=B4[:, hv * 4:(hv + 1) * 4, ti * 128:(ti + 1) * 128], op=ALU.mult),
                                 reads=[kk] + KS("B4"), writes=KS("B3"))
                    WS.release(2)
                    for half in range(2):
                        sb_ = WS.get(("w_b", l, None, half * PW))
                        sg_ = WS.get(("w_in", l, None, 5 * D + half * PW))
                        for o4 in range(4):
                            oc = half * 4 + o4
                            (pv, kv), (pg, kg) = pa_pair()
                            mm_group(pv[:, :G], kv, [(ring[:, sb_, kc, o4 * 128:(o4 + 1) * 128], B3[:, kc, :G]) for kc in range(KC)], [("ring", sb_)] + KS("B3"))
                            mm_group(pg[:, :G], kg, [(ring[:, sg_, kc, o4 * 128:(o4 + 1) * 128], B1[:, kc, :G]) for kc in range(KC)], [("ring", sg_)] + KS("B1"))
                            tb, tk = next_tmp()
                            tb2, tk2 = next_tmp()
                            S.op("act", lambda e, pg=pg, tb=tb, oc=oc: e.activation(out=tb[:, :G], in_=pg[:, :G], func=AF.Sigmoid, bias=ppc(PP_BIN + 40, oc), scale=1.0),
                                 reads=[kg, "pp"], writes=[tk])
                            S.op("dve", lambda e, pv=pv, tb=tb, tb2=tb2, oc=oc: e.scalar_tensor_tensor(out=tb2[:, :G], in0=pv[:, :G], scalar=ppc(PP_BB, oc),
                                                                                                         in1=tb[:, :G], op0=ALU.add, op1=ALU.mult),
                                 reads=[kv, tk, "pp"], writes=[tk2])
                            S.op("dve", lambda e, tb2=tb2, oc=oc: e.tensor_tensor(out=B4[:, oc, :G], in0=tb2[:, :G], in1=F2[:, oc, :G], op=ALU.add),
                                 reads=[tk2, ("F2", oc)], writes=[("B4", oc)])
                        WS.release(2)
                    for half in range(2):
                        so = WS.get(("w_out", l, None, half * PW))
                        for o4 in range(4):
                            oc = half * 4 + o4
                            po, ko = pa_one()
                            mm_group(po[:, :G], ko, [(ring[:, so, kc, o4 * 128:(o4 + 1) * 128], B4[:, kc, :G]) for kc in range(KC)], [("ring", so)] + KS("B4"))
                            tb, tk = next_tmp()
                            S.op("dve", lambda e, po=po, tb=tb, oc=oc: e.tensor_scalar(out=tb[:, :G], in0=po[:, :G], scalar1=ppc(PP_BOUT, oc), scalar2=mod[:, 16 + oc:17 + oc],
                                                                                        op0=ALU.add, op1=ALU.mult),
                                 reads=[ko, "pp", "mod"], writes=[tk])
                            S.op("dve", lambda e, tb=tb, oc=oc: e.scalar_tensor_tensor(out=F1[:, oc, :G], in0=xg[:, oc, :G], scalar=ALPHA, in1=tb[:, :G],
                                                                                        op0=ALU.mult, op1=ALU.add),
                                 reads=[tk, ("xg", oc)], writes=[("F1", oc)])
                        WS.release(1)
                    layer_norm(F1, "F1", G, lambda kc: ppc(PP_P1G, kc), lambda kc: ppc(PP_P1B, kc), AF.Identity, xg, "xg")
                    S.dma("sp", fm(dst)[:, :, c0:c0 + G], xg[:, :, :G], reads=KS("xg"), writes=[(dstn, gi)])
                for gi, (t0, nt) in enumerate(groups):
                    _grp1(gi, t0, nt)
                S.barrier()

        def moe_phase(l, srcn, src, dstn, dst, fin):
            S.barrier()
            S.new_epoch()
            with ExitStack() as ps_:
                wrb = sb("wrb", [128, KC, NE], BF16, ps_)
                brc = sb("brc", [128, NE], F32, ps_)
                bdn_sb = sb("bdn_sb", [NE, D], F32, ps_)
                htm = [sb("htm%d" % i, [128, D], BF16, ps_) for i in range(2)]
                lg = sb("lg", [128, NT, NE], F32, ps_)
                mx8 = sb("mx8", [128, NT, 8], F32, ps_)
                nmx = sb("nmx", [128, NT], F32, ps_)
                msk = sb("msk", [128, NT, NE], F32, ps_)
                mskb = sb("mskb", [128, NT, NE], BF16, ps_)
                wts = sb("wts", [128, NT, NE], F32, ps_)
                ssum = sb("ssum", [128, NT], F32, ps_)
                gsl = sb("gsl", [128, NT, NE], F32, ps_)
                gs8 = sb("gs8", [128, NT, 8], F32, ps_)
                gs4 = sb("gs4", [128, NT, 4], I32, ps_)
                w4 = sb("w4", [128, NT, 4], F32, ps_)
                eqm = sb("eqm", [128, NT, NE], F32, ps_)
                cmp5 = sb("cmp5", [NE, NTHR], F32, ps_)
                nb1 = sb("nb1", [NE, 1], F32, ps_)
                pcT = sb("pcT", [NE, 128], BF16, ps_)
                offs1 = sb("offs1", [128, NE], F32, ps_)
                cend = sb("cend", [128, NE], F32, ps_)
                escmp = sb("escmp", [128, NS, NE], F32, ps_)
                es_f = sb("es_f", [128, NS], F32, ps_)
                es_t = sb("es_t", [128, NS], F32, ps_)
                nused_i = sb("nused_i", [1, 1], I32, ps_)
                idx_gu = sb("idx_gu", [128, NS, 4], I32, ps_)
                idx_dn = sb("idx_dn", [128, NS, 2], I32, ps_)
                idx_b = sb("idx_b", [128, NS], I32, ps_)
                bgs = [sb("bgs%d" % i, [128, 16], F32, ps_) for i in range(2)]
                bu1s = [sb("bu1s%d" % i, [128, 8], F32, ps_) for i in range(2)]
                Xs = sb("Xs", [128, NSC, D], BF16, ps_)
                XT = sb("XT", [128, KC, CAP], BF16, ps_)
                ACT_ = sb("ACTb", [128, KC, CAP], BF16, ps_)
                Yb = [sb("Yb%d" % i, [128, D], F32, ps_) for i in range(2)]
                G4 = sb("G4", [128, 4, D], F32, ps_)
                ysums = [sb("ysum%d" % i, [128, D], F32, ps_) for i in range(2)]
                wtsT = sb("wtsT", [NE, 128], F32, ps_)
                PX = pst("PX", [128, D], BF16, ps_)
                PM = pst("PM", [128, 512], F32, ps_)
                PMb = PM[:].bitcast(BF16)

                def dyn_piece(kind, s, c0):
                    h = c0 // PW
                    if kind == "gu":
                        return w_gu.rearrange("l r a b -> (l r) (a b)"), idx_gu[:, s, h:h + 1]
                    if kind == "dn":
                        return w_dn.rearrange("l r a b -> (l r) (a b)"), idx_dn[:, s, h:h + 1]
                    return bgud.rearrange("l r j -> (l r) j"), idx_b[:, s:s + 1]

                S.dma("pool", wrb[:], wrd[l].rearrange("(c p) n -> p c n", p=128), writes=["wrb"])
                S.dma("sp", brc[:], bcd[l, :, BC_BR:BC_BR + NE], writes=["brc"])
                S.dma("sp", bdn_sb[:], bdnd[l], writes=["bdn"])

                def _r1(gi, t0, nt):
                    G = nt * 128
                    c0 = t0 * 128
                    S.dma("sp", xg[:, :, :G], fm(src)[:, :, c0:c0 + G], reads=[(srcn, gi)], writes=KS("xg"))
                    layer_norm(xg, "xg", G, lambda kc: scp[:, 8 + kc:9 + kc], lambda kc: mod[:, 24 + kc:25 + kc], AF.Identity, B1, "B1")
                    for ti in range(nt):
                        T = t0 + ti
                        mm_group(PM[:, 0:NE], "PM", [(B1[:, kc, ti * 128:(ti + 1) * 128], wrb[:, kc, :]) for kc in range(KC)], KS("B1") + ["wrb"])
                        S.op("dve", lambda e, T=T: e.tensor_tensor(out=lg[:, T, :], in0=PM[:, 0:NE], in1=brc[:], op=ALU.add), reads=["PM", "brc"], writes=[("lg", T)])
                        for kc in range(KC):
                            S.op("pe", lambda e, kc=kc, ti=ti: e.transpose(out=PX[:, kc * 128:(kc + 1) * 128], in_=B1[:, kc, ti * 128:(ti + 1) * 128], identity=identb[:]),
                                 reads=[("B1", kc), "identb"], writes=["PX", ("PXh", 0), ("PXh", 1)])
                        hb = htm[T % 2]
                        S.op("act", lambda e, hb=hb: e.activation(out=hb[:], in_=PX[:], func=AF.Copy), reads=["PX", ("PXh", 0), ("PXh", 1)], writes=[("htm", T % 2)])
                        S.dma("sp", Hd[T * 128:(T + 1) * 128, :], hb[:], reads=[("htm", T % 2)], writes=[("H", T)])
                for gi, (t0, nt) in enumerate(groups):
                    _r1(gi, t0, nt)

                for T in range(NT):
                    S.op("dve", lambda e, T=T: e.max(out=mx8[:, T, :], in_=lg[:, T, :]), reads=[("lg", T)], writes=[("mx8", T)])
                    S.op("dve", lambda e, T=T: e.tensor_scalar(out=msk[:, T, :], in0=lg[:, T, :], scalar1=mx8[:, T, 3:4], scalar2=None, op0=ALU.is_ge),
                         reads=[("lg", T), ("mx8", T)], writes=[("msk", T)])
                MXA = [("mx8", T) for T in range(NT)]
                MSA = [("msk", T) for T in range(NT)]
                S.op("dve", lambda e: e.tensor_scalar(out=nmx[:], in0=mx8[:, :, 0], scalar1=-1.0, scalar2=None, op0=ALU.mult), reads=MXA, writes=["nmx"])
                for T in range(NT):
                    S.op("act", lambda e, T=T: e.activation(out=wts[:, T, :], in_=lg[:, T, :], func=AF.Exp, bias=nmx[:, T:T + 1], scale=1.0),
                         reads=[("lg", T), "nmx"], writes=[("wts", T)])
                WTA = [("wts", T) for T in range(NT)]
                S.op("dve", lambda e: e.tensor_tensor(out=wts[:], in0=wts[:], in1=msk[:], op=ALU.mult), reads=WTA + MSA, writes=WTA)
                S.op("dve", lambda e: e.tensor_reduce(out=ssum[:], in_=wts[:], axis=mybir.AxisListType.X, op=ALU.add), reads=WTA, writes=["ssum"])
                S.op("dve", lambda e: e.reciprocal(out=ssum[:], in_=ssum[:]), reads=["ssum"], writes=["ssum"])
                S.op("dve", lambda e: e.tensor_tensor(out=wts[:], in0=wts[:], in1=ssum[:, :, None].to_broadcast([128, NT, NE]), op=ALU.mult),
                     reads=WTA + ["ssum"], writes=WTA)
                S.op("dve", lambda e: e.tensor_copy(out=mskb[:], in_=msk[:]), reads=MSA, writes=["mskb"])
                mm_group(PM[0:NE, 128:256], "PMc", [(mskb[:, T, :], onesb[:]) for T in range(NT)], ["mskb", "onesb"])
                S.op("dve", lambda e: e.tensor_scalar(out=cmp5[:], in0=cst_sb[0:NE, CL["thr5"][0]:CL["thr5"][1]], scalar1=PM[0:NE, 128:129], scalar2=None, op0=ALU.is_lt),
                     reads=["PMc", "cst"], writes=["cmp5"])
                S.op("dve", lambda e: e.tensor_reduce(out=nb1[:], in_=cmp5[:], axis=mybir.AxisListType.X, op=ALU.add), reads=["cmp5"], writes=["nb1"])
                S.op("dve", lambda e: e.tensor_scalar(out=pcT[:], in0=onesb[0:NE, :], scalar1=nb1[:, 0:1], scalar2=float(CAP), op0=ALU.mult, op1=ALU.mult),
                     reads=["onesb", "nb1"], writes=["pcT"])
                S.op("pe", lambda e: e.matmul(PM[:, 256:256 + NE], lhsT=pcT[:], rhs=uexb[0:NE, :], start=True, stop=True), reads=["pcT", "uexb"], writes=["PMo"])
                S.op("pe", lambda e: e.matmul(PM[:, 288:288 + NE], lhsT=pcT[:], rhs=uinb[0:NE, :], start=True, stop=True), reads=["pcT", "uinb"], writes=["PMo"])
                S.op("dve", lambda e: e.tensor_scalar(out=offs1[:], in0=PM[:, 256:256 + NE], scalar1=1.0, scalar2=None, op0=ALU.add), reads=["PMo"], writes=["offs1"])
                S.op("act", lambda e: e.activation(out=cend[:], in_=PM[:, 288:288 + NE], func=AF.Copy), reads=["PMo"], writes=["cend"])
                S.op("dve", lambda e: e.tensor_tensor(out=escmp[:], in0=cend[:, None, :].to_broadcast([128, NS, NE]),
                                                       in1=cst_sb[:, CL["sthr"][0]:CL["sthr"][1], None].to_broadcast([128, NS, NE]), op=ALU.is_le),
                     reads=["cend", "cst"], writes=["escmp"])
                S.op("dve", lambda e: e.tensor_reduce(out=es_f[:], in_=escmp[:], axis=mybir.AxisListType.X, op=ALU.add), reads=["escmp"], writes=["es_f"])
                S.op("dve", lambda e: e.tensor_scalar(out=es_f[:], in0=es_f[:], scalar1=float(NE - 1), scalar2=0.0, op0=ALU.min, op1=ALU.max), reads=["es_f"], writes=["es_f"])
                def mkidx(out_ap, mult, base, h):
                    S.op("dve", lambda e: e.tensor_scalar(out=es_t[:], in0=es_f[:], scalar1=float(mult), scalar2=float(base), op0=ALU.mult, op1=ALU.add),
                         reads=["es_f"], writes=["es_t"])
                    S.op("dve", lambda e: e.tensor_tensor(out=out_ap, in0=es_t[:], in1=cst_sb[:, CL["pidx"][0] + h:CL["pidx"][0] + h + 1].to_broadcast([128, NS]), op=ALU.add),
                         reads=["es_t", "cst"], writes=["es_i"])
                for h in range(4):
                    mkidx(idx_gu[:, :, h], 512, l * NE * 4 * 128, h)
                for h in range(2):
                    mkidx(idx_dn[:, :, h], 256, l * NE * 2 * 128, h)
                mkidx(idx_b[:], 128, l * NE * 128, 0)
                WS.enable_dyn(l, lambda desc: dyn_piece(desc[3], desc[2], desc[4]))
                for T in range(NT):
                    pairs = [(onesb[:], mskb[:, T2, :]) for T2 in range(T)] + [(ltrib[:], mskb[:, T, :])]
                    mm_group(PM[:, 0:NE], "PM", pairs, ["onesb", "ltrib", "mskb"])
                    S.op("dve", lambda e, T=T: e.tensor_tensor(out=gsl[:, T, :], in0=PM[:, 0:NE], in1=offs1[:], op=ALU.add), reads=["PM", "offs1"], writes=[("gsl", T)])
                    S.op("dve", lambda e, T=T: e.tensor_tensor(out=gsl[:, T, :], in0=gsl[:, T, :], in1=msk[:, T, :], op=ALU.mult), reads=[("gsl", T), ("msk", T)], writes=[("gsl", T)])
                    S.op("dve", lambda e, T=T: e.max(out=gs8[:, T, :], in_=gsl[:, T, :]), reads=[("gsl", T)], writes=[("gs8", T)])
                GSA = [("gsl", T) for T in range(NT)]
                G8A = [("gs8", T) for T in range(NT)]
                S.op("dve", lambda e: e.tensor_scalar(out=gs4[:], in0=gs8[:, :, 0:4], scalar1=-1.0, scalar2=None, op0=ALU.add), reads=G8A, writes=["gs4"])
                for k in range(4):
                    S.op("dve", lambda e, k=k: e.tensor_tensor(out=eqm[:], in0=gsl[:], in1=gs8[:, :, k:k + 1].to_broadcast([128, NT, NE]), op=ALU.is_equal),
                         reads=GSA + G8A, writes=["eqm"])
                    S.op("dve", lambda e: e.tensor_tensor(out=eqm[:], in0=eqm[:], in1=wts[:], op=ALU.mult), reads=["eqm"] + WTA, writes=["eqm"])
                    S.op("dve", lambda e, k=k: e.tensor_reduce(out=w4[:, :, k], in_=eqm[:], axis=mybir.AxisListType.X, op=ALU.add), reads=["eqm"], writes=[("w4", k)])
                W4A = [("w4", k) for k in range(4)]
                S.op("dve", lambda e: e.tensor_scalar(out=w4[:], in0=w4[:], scalar1=1.0 / SW_A, scalar2=None, op0=ALU.mult), reads=W4A, writes=W4A)

                for T in range(NT):
                    hb = htm[T % 2]
                    S.dma("sp", hb[:], Hd[T * 128:(T + 1) * 128, :], reads=[("H", T)], writes=[("htm", T % 2)])
                    for k in range(4):
                        S.op("pool", lambda e, hb=hb, T=T, k=k: e.indirect_dma_start(out=Xd, out_offset=bass.IndirectOffsetOnAxis(ap=gs4[:, T, k:k + 1], axis=0),
                                                                                      in_=hb[:], in_offset=None),
                             reads=[("htm", T % 2), "gs4"], writes=[("Xd", T, k)], dma=True)

                XDALL = [("Xd", T, k) for T in range(NT) for k in range(4)]

                def load_x(s):
                    S.dma("sp", Xs[:], Xd[s * CAP:(s + 1) * CAP, :].rearrange("(c p) d -> p c d", p=128), reads=XDALL, writes=[("Xs", sc) for sc in range(NSC)])
                    bg = bgs[s % 2]
                    btab, bidx = dyn_piece("b", s, 0)
                    S.op("pool", lambda e, bg=bg, btab=btab, bidx=bidx: e.indirect_dma_start(out=bg[:], out_offset=None, in_=btab,
                                                                                             in_offset=bass.IndirectOffsetOnAxis(ap=bidx, axis=0)),
                         reads=["es_i"], writes=[("bgs", s % 2)], dma=True)
                    b1 = bu1s[s % 2]
                    S.op("dve", lambda e, bg=bg, b1=b1: e.tensor_scalar(out=b1[:], in0=bg[:, 8:16], scalar1=1.0, scalar2=None, op0=ALU.add),
                         reads=[("bgs", s % 2)], writes=[("bu1s", s % 2)])

                def _step(s):
                    bg = bgs[s % 2]
                    b1 = bu1s[s % 2]
                    for kc in range(KC):
                        pxb = PX if kc % 2 == 0 else PMb
                        pxk = [("PXh", 0), ("PXh", 1), "PX"] if kc % 2 == 0 else ["PM", "PMc", "PMo"]
                        for sc in range(NSC):
                            S.op("pe", lambda e, kc=kc, sc=sc, pxb=pxb: e.transpose(out=pxb[:, sc * 128:(sc + 1) * 128], in_=Xs[:, sc, kc * 128:(kc + 1) * 128], identity=identb[:]),
                                 reads=[("Xs", sc), "identb"], writes=pxk)
                        if kc % 2 == 0:
                            S.op("act", lambda e, kc=kc, pxb=pxb: e.activation(out=XT[:, kc, :], in_=pxb[:, 0:CAP], func=AF.Copy), reads=pxk, writes=[("XT", kc)])
                        else:
                            S.op("dve", lambda e, kc=kc, pxb=pxb: e.tensor_copy(out=XT[:, kc, :], in_=pxb[:, 0:CAP]), reads=pxk, writes=[("XT", kc)])
                    if s + 1 < NS:
                        load_x(s + 1)
                    for half in range(2):
                        sg_ = WS.get(("dyn", l, s, "gu", half * PW))
                        su = WS.get(("dyn", l, s, "gu", D + half * PW))
                        for o4 in range(4):
                            j = half * 4 + o4
                            (pg, kg), (pu, ku) = pa_pair()
                            mm_group(pg[:, :CAP], kg, [(ring[:, sg_, kc, o4 * 128:(o4 + 1) * 128], XT[:, kc, :]) for kc in range(KC)], [("ring", sg_)] + KS("XT"))
                            mm_group(pu[:, :CAP], ku, [(ring[:, su, kc, o4 * 128:(o4 + 1) * 128], XT[:, kc, :]) for kc in range(KC)], [("ring", su)] + KS("XT"))
                            tg, tgk = next_tmp()
                            ts_, tsk = next_tmp()
                            tu, tuk = next_tmp()
                            S.op("dve", lambda e, pg=pg, tg=tg, j=j, bg=bg: e.tensor_scalar(out=tg[:, :CAP], in0=pg[:, :CAP], scalar1=bg[:, j:j + 1], scalar2=SW_L, op0=ALU.add, op1=ALU.min),
                                 reads=[kg, ("bgs", s % 2)], writes=[tgk])
                            S.op("act", lambda e, tg=tg, ts_=ts_: e.activation(out=ts_[:, :CAP], in_=tg[:, :CAP], func=AF.Silu, scale=SW_A), reads=[tgk], writes=[tsk])
                            S.op("dve", lambda e, pu=pu, tu=tu, j=j, b1=b1: e.tensor_scalar(out=tu[:, :CAP], in0=pu[:, :CAP], scalar1=b1[:, j:j + 1], scalar2=1.0 - SW_L, op0=ALU.add, op1=ALU.max),
                                 reads=[ku, ("bu1s", s % 2)], writes=[tuk])
                            S.op("dve", lambda e, tu=tu, ts_=ts_, j=j: e.scalar_tensor_tensor(out=ACT_[:, j, :], in0=tu[:, :CAP], scalar=1.0 + SW_L, in1=ts_[:, :CAP], op0=ALU.min, op1=ALU.mult),
                                 reads=[tuk, tsk], writes=[("ACT", j)])
                        WS.release(2)
                    sd0 = WS.get(("dyn", l, s, "dn", 0))
                    sd1 = WS.get(("dyn", l, s, "dn", PW))
                    for sc in range(NSC):
                        yb = Yb[sc % 2]
                        for hd, sd in enumerate((sd0, sd1)):
                            pd, kd = pa_one()
                            mm_group(pd[:], kd, [(ACT_[:, j, sc * 128:(sc + 1) * 128], ring[:, sd, j, :]) for j in range(KC)], [("ring", sd)] + KS("ACT"))
                            if hd == 0:
                                S.op("act", lambda e, pd=pd, yb=yb: e.activation(out=yb[:, 0:512], in_=pd[:], func=AF.Copy), reads=[kd], writes=[("Yb", sc % 2, 0)])
                            else:
                                S.op("dve", lambda e, pd=pd, yb=yb: e.tensor_copy(out=yb[:, 512:1024], in_=pd[:]), reads=[kd], writes=[("Yb", sc % 2, 1)])
                        S.dma("sp", Yd[s * CAP + sc * 128:s * CAP + (sc + 1) * 128, :], yb[:], reads=[("Yb", sc % 2, 0), ("Yb", sc % 2, 1)], writes=[("Y", s, sc)])
                    WS.release(2)
                S.op("dve", lambda e: e.tensor_scalar(out=nused_i[:], in0=cend[0:1, NE - 1:NE], scalar1=1.0 / CAP, scalar2=0.25, op0=ALU.mult, op1=ALU.add),
                     reads=["cend"], writes=["nused"])
                for en in S.ENGS:
                    S.op(en, lambda e, en=en: e.reg_load(S.regs[en], nused_i[0:1, 0:1]), reads=["nused"], writes=[("nused_reg", en)])
                load_x(0)
                WS.hold_static = True
                for s in range(NS):
                    if s >= NMIN:
                        S.cur_region = (("moe", l, s), s)
                    _step(s)
                S.cur_region = None
                WS.hold_static = False
                WS.release(0)

                YALL = [("Y", s, sc) for s in range(NS) for sc in range(NSC)]

                def _r4(gi, t0, nt):
                    G = nt * 128
                    c0 = t0 * 128
                    S.dma("sp", xg[:, :, :G], fm(src)[:, :, c0:c0 + G], reads=[(srcn, gi)], writes=KS("xg"))
                    S.op("act", lambda e: e.activation(out=xg[:, :, :G], in_=xg[:, :, :G], func=AF.Copy, scale=ALPHA), reads=KS("xg"), writes=KS("xg"))
                    for ti in range(nt):
                        T = t0 + ti
                        for k in range(4):
                            S.op("pool", lambda e, T=T, k=k: e.indirect_dma_start(out=G4[:, k, :], out_offset=None, in_=Yd,
                                                                                   in_offset=bass.IndirectOffsetOnAxis(ap=gs4[:, T, k:k + 1], axis=0)),
                                 reads=YALL + ["gs4"], writes=[("G4", k)], dma=True)
                        ysum = ysums[T % 2]
                        yk = ("ysum", T % 2)
                        pbase = (T % 2) * 2
                        S.op("dve", lambda e, T=T, ysum=ysum: e.tensor_scalar(out=ysum[:], in0=G4[:, 0, :], scalar1=w4[:, T, 0:1], scalar2=None, op0=ALU.mult),
                             reads=[("G4", 0)] + W4A, writes=[yk])
                        for k in range(1, 4):
                            S.op("dve", lambda e, T=T, k=k, ysum=ysum: e.scalar_tensor_tensor(out=ysum[:], in0=G4[:, k, :], scalar=w4[:, T, k:k + 1], in1=ysum[:], op0=ALU.mult, op1=ALU.add),
                                 reads=[("G4", k), yk] + W4A, writes=[yk])
                        S.op("pe", lambda e, T=T: e.transpose(out=PM[0:NE, 128:256], in_=wts[:, T, :], identity=ident_f), reads=[("wts", T), "cst"], writes=["PMc"])
                        S.op("act", lambda e: e.activation(out=wtsT[:], in_=PM[0:NE, 128:256], func=AF.Copy), reads=["PMc"], writes=["wtsT"])
                        for kc in range(KC):
                            pq = PA[pbase + kc // 4]
                            pk = ("PA", pbase + kc // 4)
                            S.op("pe", lambda e, kc=kc, pq=pq, ysum=ysum: e.matmul(pq[:, (kc % 4) * 128:(kc % 4 + 1) * 128], lhsT=ysum[:, kc * 128:(kc + 1) * 128], rhs=ident_f, start=True, stop=False),
                                 reads=[yk, "cst"], writes=[pk])
                            S.op("pe", lambda e, kc=kc, pq=pq: e.matmul(pq[:, (kc % 4) * 128:(kc % 4 + 1) * 128], lhsT=bdn_sb[:, kc * 128:(kc + 1) * 128], rhs=wtsT[:], start=False, stop=True),
                                 reads=["bdn", "wtsT"], writes=[pk])
                        for kc in range(KC):
                            pq = PA[pbase + kc // 4]
                            pk = ("PA", pbase + kc // 4)
                            S.op("dve", lambda e, kc=kc, ti=ti, pq=pq: e.scalar_tensor_tensor(out=F1[:, kc, ti * 128:(ti + 1) * 128], in0=pq[:, (kc % 4) * 128:(kc % 4 + 1) * 128],
                                                                                               scalar=mod[:, 40 + kc:41 + kc], in1=xg[:, kc, ti * 128:(ti + 1) * 128], op0=ALU.mult, op1=ALU.add),
                                 reads=[pk, "mod", ("xg", kc)], writes=[("F1", kc)])
                    layer_norm(F1, "F1", G, lambda kc: ppc(PP_P2G, kc), lambda kc: ppc(PP_P2B, kc), AF.Identity, xg, "xg")
                    d_ = S.dma("sp", fm(dst)[:, :, c0:c0 + G], xg[:, :, :G], reads=KS("xg"), writes=[(dstn, gi)])
                    if fin is not None:
                        fin.append(d_)
                for gi, (t0, nt) in enumerate(groups):
                    _r4(gi, t0, nt)
                if DEBUG:
                    dd = []
                    dd.append(S.dma("sp", dbg[:, 0:NS], es_f[:], reads=["es_f"], writes=["dbg0"]))
                    dd.append(S.dma("sp", dbg[:, 64:96], offs1[:], reads=["offs1"], writes=["dbg1"]))
                    dd.append(S.dma("sp", dbg[:, 96:128], cend[:], reads=["cend"], writes=["dbg2"]))
                    dd.append(S.dma("sp", dbg[:, 128:128 + NT * 8], gs8[:].rearrange("p a b -> p (a b)"), reads=G8A, writes=["dbg3"]))
                    dd.append(S.dma("sp", dbg[:, 512:512 + NT * 4], w4[:].rearrange("p a b -> p (a b)"), reads=W4A, writes=["dbg4"]))
                    dd.append(S.dma("sp", dbg[:, 1024:1024 + NT * NE], wts[:].rearrange("p a b -> p (a b)"), reads=WTA, writes=["dbg5"]))
                    dd.append(S.dma("sp", dbg[:, 2048:2048 + NT * NE], gsl[:].rearrange("p a b -> p (a b)"), reads=GSA, writes=["dbg6"]))
                    fin.extend(dd)
                S.barrier()

        fin_dmas = []
        for l in range(L):
            S.barrier()
            S.dma("sp", pp[:], ppd[l], writes=["pp"])
            PMOD = PA[0]
            for hp in range(12):
                s = WS.get(("w_mod", l, None, hp * PW))
                for oc in range(4):
                    col = hp * 4 + oc
                    for kc in range(KC):
                        S.op("pe", lambda e, s=s, oc=oc, kc=kc, col=col: e.matmul(PMOD[:, col:col + 1], lhsT=ring[:, s, kc, oc * 128:(oc + 1) * 128],
                                                                                  rhs=cond[:, kc:kc + 1], start=(kc == 0), stop=(kc == KC - 1)),
                             reads=[("ring", s), "cond"], writes=[("PA", 0)])
                WS.release()
            S.op("dve", lambda e: e.tensor_tensor(out=mod[:], in0=PMOD[:, 0:48], in1=pp[:, PP_BMOD:PP_BMOD + 48], op=ALU.add),
                 reads=[("PA", 0), "pp"], writes=["mod"])
            S.op("dve", lambda e: e.tensor_scalar(out=scp[:, 0:8], in0=mod[:, 8:16], scalar1=1.0, scalar2=None, op0=ALU.add), reads=["mod"], writes=["mod"])
            S.op("dve", lambda e: e.tensor_scalar(out=scp[:, 8:16], in0=mod[:, 32:40], scalar1=1.0, scalar2=None, op0=ALU.add), reads=["mod"], writes=["mod"])

            srcn, src = chain[2 * l]
            dstn, dst = chain[2 * l + 1]
            mixer_phase(l, srcn, src, dstn, dst)
            srcn, src = chain[2 * l + 1]
            dstn, dst = chain[2 * l + 2]
            moe_phase(l, srcn, src, dstn, dst, fin_dmas if l == L - 1 else None)
        S.final_wait("sp", fin_dmas)
        if plan is not None:
            S.emit()
    return nc, WS.collected


def _vec8(v):
    return np.ascontiguousarray(v.reshape(-1, 128).T)


def _consts(NT):
    CL, NCST = cst_layout(NT)
    c = np.zeros((128, NCST), np.float32)
    c[:, CL["ident"][0]:CL["ident"][1]] = np.eye(128, dtype=np.float32)
    c[:, CL["ltri"][0]:CL["ltri"][1]] = np.triu(np.ones((128, 128), np.float32), 1)
    c[:, CL["thr5"][0]:CL["thr5"][1]] = (np.arange(NTHR, dtype=np.float32) * CAP)[None, :]
    c[:NE, CL["uex"][0]:CL["uex"][1]] = np.triu(np.ones((NE, NE), np.float32), 1)
    c[:NE, CL["uin"][0]:CL["uin"][1]] = np.triu(np.ones((NE, NE), np.float32), 0)
    c[:, CL["sthr"][0]:CL["sthr"][1]] = (np.arange((4 * NT * 128 + CAP - 1) // CAP + NE, dtype=np.float32) * CAP)[None, :]
    c[:, CL["pidx"][0]:CL["pidx"][1]] = np.arange(128, dtype=np.float32)[:, None] + 128.0 * np.arange(4, dtype=np.float32)[None, :]
    return c


def _layer_params(inp, ls):
    Ln = len(ls)
    pp = np.zeros((Ln, 128, NPP), np.float32)
    bc = np.zeros((Ln, 128, NBC), np.float32)
    for i, l in enumerate(ls):
        pp[i, :, PP_BIN:PP_BIN + 48] = _vec8(inp["b_in"][l])
        pp[i, :, PP_CONVW:PP_CONVW + 248] = inp["conv_w"][l].reshape(CONVW, 8, 128).transpose(2, 0, 1).reshape(128, 248)
        pp[i, :, PP_CONVB:PP_CONVB + 8] = _vec8(inp["conv_b"][l])
        pp[i, :, PP_LNAG:PP_LNAG + 8] = _vec8(inp["ln_a_g"][l])
        pp[i, :, PP_LNAB:PP_LNAB + 8] = _vec8(inp["ln_a_b"][l])
        pp[i, :, PP_BA:PP_BA + 8] = _vec8(inp["b_a"][l])
        pp[i, :, PP_BB:PP_BB + 8] = _vec8(inp["b_b"][l])
        pp[i, :, PP_BOUT:PP_BOUT + 8] = _vec8(inp["b_out"][l])
        pp[i, :, PP_P1G:PP_P1G + 8] = _vec8(inp["post1_g"][l])
        pp[i, :, PP_P1B:PP_P1B + 8] = _vec8(inp["post1_b"][l])
        pp[i, :, PP_P2G:PP_P2G + 8] = _vec8(inp["post2_g"][l])
        pp[i, :, PP_P2B:PP_P2B + 8] = _vec8(inp["post2_b"][l])
        pp[i, :, PP_BMOD:PP_BMOD + 48] = _vec8(inp["b_mod"][l])
        pp[i, :, PP_BGU:PP_BGU + NE * 16] = inp["b_gu"][l].reshape(NE, 16, 128).transpose(2, 0, 1).reshape(128, NE * 16)
        bc[i, :, BC_BV:BC_BV + D] = inp["b_in"][l][3 * D:4 * D][None, :]
        bc[i, :, BC_LVG:BC_LVG + D] = inp["ln_v_g"][l][None, :]
        bc[i, :, BC_LVB:BC_LVB + D] = inp["ln_v_b"][l][None, :]
        bc[i, :, BC_BR:BC_BR + NE] = inp["b_router"][l][None, :]
    ls = list(ls)
    out = {
        "pp": pp, "bc": bc,
        "bdn": np.ascontiguousarray(inp["b_dn"][ls]),
        "bs": np.ascontiguousarray(inp["b_s"][ls].reshape(Ln, 1, D)),
        "wsT": np.ascontiguousarray(inp["w_s"][ls].transpose(0, 3, 1, 2)),
        "wr": np.ascontiguousarray(inp["w_router"][ls]),
        "bgu": np.ascontiguousarray(inp["b_gu"][ls].reshape(Ln, NE, 16, 128).transpose(0, 1, 3, 2)).reshape(Ln, NE * 128, 16),
        "w_gu": np.ascontiguousarray(inp["w_gu"][ls].reshape(Ln, NE, KC, 128, 4, PW).transpose(0, 1, 4, 3, 2, 5)).reshape(Ln, NE * 4 * 128, KC, PW),
        "w_dn": np.ascontiguousarray(inp["w_dn"][ls].reshape(Ln, NE, KC, 128, 2, PW).transpose(0, 1, 4, 3, 2, 5)).reshape(Ln, NE * 2 * 128, KC, PW),
    }
    for k in ("w_mod", "w_in", "w_a", "w_b", "w_out"):
        out[k] = inp[k][ls[0]:ls[-1] + 1]
    return out


def _shard_x(x, NH, cores):
    NT = TPC // 128 + NH
    outs = []
    for cid in cores:
        b, half = cid // 2, cid % 2
        start = half * TPC
        xt = np.zeros((D, NT * 128), np.float32)
        xt[:, NH * 128:] = x[b, start:start + TPC, :].T
        if half == 1 and NH > 0:
            xt[:, :NH * 128] = x[b, start - NH * 128:start, :].T
        outs.append(xt)
    return outs


_PROG_CACHE = {}


def _get_prog(NH, L):
    key = (NH, L)
    if key not in _PROG_CACHE:
        _, plan = build_program(NH, L, plan=None)
        nc, _ = build_program(NH, L, plan=plan)
        _PROG_CACHE[key] = nc
    return _PROG_CACHE[key]


def run_layers(inp, x, ls, NH, cores):
    NT = TPC // 128 + NH
    nc = _get_prog(NH, len(ls))
    lp = _layer_params(inp, ls)
    cst = _consts(NT)
    xs = _shard_x(x, NH, cores)
    in_maps = []
    for i, cid in enumerate(cores):
        b, half = cid // 2, cid % 2
        m = dict(lp)
        m["xT"] = xs[i]
        m["cvec"] = _vec8(inp["c"][b])
        m["flag"] = np.full((128, 1), float(half), np.float32)
        m["cst"] = cst
        in_maps.append(m)
    res = run_bass_kernel_spmd(nc, in_maps, core_ids=list(range(len(cores))))
    xn = np.zeros_like(x)
    for i, cid in enumerate(cores):
        b, half = cid // 2, cid % 2
        yT = res.results[i]["yT"]
        xn[b, half * TPC:(half + 1) * TPC, :] = yT[:, NH * 128:].T
    return xn


FUSED = True


def kernel(**inputs):
    inp = {k: np.asarray(v) for k, v in inputs.items()}
    x = np.ascontiguousarray(inp["x"], dtype=np.float32)
    cores = list(range(8))
    if FUSED:
        x = run_layers(inp, x, list(range(4)), 2, cores)
    else:
        for l in range(4):
            x = run_layers(inp, x, [l], 1, cores)
    return x.astype(np.float32)
```
